# Optimizing a Trainium2 kernel written in Bass

```python
import jax, jax.numpy as jnp
from jax import lax
import numpy as np

D_MODEL = 2048
BATCH = 8
SEQ = 2048
DEPTH = 1

GRID_W = 64
CTX_LEN = 256
D_MIX = D_MODEL
GLA_WIDTH = D_MIX // 2
GLA_HEADS = 4
GLA_DK = GLA_WIDTH // 2 // GLA_HEADS
GLA_DV = GLA_WIDTH // GLA_HEADS
GLA_KEY_WIDTH = GLA_HEADS * GLA_DK
GLA_RANK = 16
GLA_TAU = 16.0
GLA_CHUNK = 64
POOL_WIDTH = D_MIX - GLA_WIDTH
POOL_WINDOWS = (2, 4, 8, 16)
POOL_GROUP = POOL_WIDTH // len(POOL_WINDOWS)
D_IN = 2 * GLA_KEY_WIDTH + 2 * GLA_WIDTH + 2 * GLA_RANK + POOL_WIDTH
SPLITS = (
    GLA_KEY_WIDTH,
    2 * GLA_KEY_WIDTH,
    2 * GLA_KEY_WIDTH + GLA_WIDTH,
    2 * GLA_KEY_WIDTH + 2 * GLA_WIDTH,
    2 * GLA_KEY_WIDTH + 2 * GLA_WIDTH + GLA_RANK,
    2 * GLA_KEY_WIDTH + 2 * GLA_WIDTH + 2 * GLA_RANK,
)
N_EXPERTS = 64
TOP_K = 8
N_GROUPS = 8
TOPK_GROUPS = 4
D_EXPERT = D_MODEL // 4
D_SHARED = D_MODEL // 4
ROUTED_SCALE = 2.5
MOE_BLOCK = 256
EPS = 1e-6

kernel_name = "hybrid_gla_pool_moe_dit_prefix"


def _rmsnorm(x, g):
    xf = x.astype(jnp.float32)
    y = xf * lax.rsqrt(jnp.mean(xf * xf, axis=-1, keepdims=True) + EPS)
    return (y * g.astype(jnp.float32)).astype(x.dtype)


def _log_decay(a_lr, w_a2, b_a):
    B, L, _ = a_lr.shape
    z = (a_lr @ w_a2 + b_a).astype(jnp.float32)
    return (jax.nn.log_sigmoid(z) / GLA_TAU).reshape(B, L, GLA_HEADS, GLA_DK)


def _gla_scan(q, k, v, g, s0, with_output):
    B, L, H, DK = q.shape
    DV = v.shape[-1]
    C = GLA_CHUNK
    n = L // C
    qc = q.reshape(B, n, C, H, DK)
    kc = k.reshape(B, n, C, H, DK)
    vc = v.reshape(B, n, C, H, DV)
    G = jnp.cumsum(g.reshape(B, n, C, H, DK), axis=2)
    G_end = G[:, :, -1:]
    u = jnp.einsum("bnchk,bnchv->nbhkv", kc * jnp.exp(G_end - G), vc)
    decay = jnp.moveaxis(jnp.exp(G_end[:, :, 0]), 1, 0)

    def step(s, inp):
        d, du = inp
        return d[..., None] * s + du, s

    s_final, s_prev = lax.scan(step, s0, (decay, u))
    if not with_output:
        return None, s_final
    o_inter = jnp.einsum("bnchk,nbhkv->bnchv", qc * jnp.exp(G), s_prev)
    G_mid = G[:, :, C // 2 : C // 2 + 1]
    a = jnp.einsum("bnihk,bnjhk->bnhij", qc * jnp.exp(G - G_mid), kc * jnp.exp(G_mid - G))
    lower = jnp.tril(jnp.ones((C, C), dtype=bool))
    a = jnp.where(lower, a, 0.0)
    o_intra = jnp.einsum("bnhij,bnjhv->bnihv", a, vc)
    return (o_inter + o_intra).reshape(B, L, H, DV), s_final


def _window_mean(x, w, axis):
    n = x.shape[axis]
    xf = jnp.moveaxis(x.astype(jnp.float32), axis, 0)
    cs = jnp.concatenate([jnp.zeros_like(xf[:1]), jnp.cumsum(xf, axis=0)], axis=0)
    pos = jnp.arange(n)
    lo = jnp.maximum(pos - w // 2, 0)
    hi = jnp.minimum(pos + w // 2, n)
    cnt = (hi - lo).astype(jnp.float32).reshape((n,) + (1,) * (xf.ndim - 1))
    return jnp.moveaxis((cs[hi] - cs[lo]) / cnt, 0, axis)


def _pool_mixer(p, w_pool, pool_scale, grid):
    B, L, _ = p.shape
    pg = p.reshape(B, L, len(POOL_WINDOWS), POOL_GROUP)
    outs = []
    for gi, w in enumerate(POOL_WINDOWS):
        xg = pg[:, :, gi]
        if grid:
            rows = L // GRID_W
            x2 = xg.reshape(B, rows, GRID_W, POOL_GROUP)
            m = _window_mean(_window_mean(x2, w, 1), w, 2).reshape(B, L, POOL_GROUP)
        else:
            m = _window_mean(xg, w, 1)
        outs.append(m.astype(p.dtype) - xg)
    d = jnp.stack(outs, axis=2)
    y = jnp.einsum("blgc,gcd->blgd", d, w_pool).reshape(B, L, POOL_WIDTH)
    return y * pool_scale


def _token_mixer(u, s_f0, s_b0, w_a2_f, b_a_f, w_a2_b, b_a_b, gla_norm, w_pool, pool_scale,
                 w_out, grid, with_output):
    B, L, _ = u.shape
    f32 = jnp.float32
    q, k, v, r, a_f, a_b, p = jnp.split(u, SPLITS, axis=-1)
    q = q.astype(f32).reshape(B, L, GLA_HEADS, GLA_DK) * (GLA_DK ** -0.5)
    k = k.astype(f32).reshape(B, L, GLA_HEADS, GLA_DK)
    v = v.astype(f32).reshape(B, L, GLA_HEADS, GLA_DV)
    g_f = _log_decay(a_f, w_a2_f, b_a_f)
    g_b = _log_decay(a_b, w_a2_b, b_a_b)
    o_f, s_f = _gla_scan(q, k, v, g_f, s_f0, with_output)
    o_b, s_b = _gla_scan(q[:, ::-1], k[:, ::-1], v[:, ::-1], g_b[:, ::-1], s_b0, with_output)
    if not with_output:
        return None, s_f, s_b
    o = o_f + o_b[:, ::-1]
    o = o * lax.rsqrt(jnp.mean(o * o, axis=-1, keepdims=True) + EPS)
    o = o * gla_norm.astype(f32).reshape(GLA_HEADS, GLA_DV)
    y_gla = o.reshape(B, L, GLA_WIDTH).astype(u.dtype) * jax.nn.silu(r)
    y_pool = _pool_mixer(p, w_pool, pool_scale, grid)
    y = jnp.concatenate([y_gla, y_pool], axis=-1) @ w_out
    return y, s_f, s_b


def _route(h, w_router, router_bias):
    scores = jax.nn.sigmoid((h @ w_router).astype(jnp.float32))
    sel = scores + router_bias.astype(jnp.float32)
    grp = sel.reshape(-1, N_GROUPS, N_EXPERTS // N_GROUPS)
    grp_score = lax.top_k(grp, 2)[0].sum(-1)
    _, gidx = lax.top_k(grp_score, TOPK_GROUPS)
    gmask = jax.nn.one_hot(gidx, N_GROUPS, dtype=jnp.float32).sum(1) > 0
    emask = jnp.repeat(gmask, N_EXPERTS // N_GROUPS, axis=1)
    _, eidx = lax.top_k(jnp.where(emask, sel, -jnp.inf), TOP_K)
    wts = jnp.take_along_axis(scores, eidx, axis=1)
    wts = wts / jnp.sum(wts, axis=-1, keepdims=True) * ROUTED_SCALE
    return eidx, wts


def _moe(h, w_router, router_bias, w_eg, w_eu, w_ed, w_sg, w_su, w_sd):
    T, D = h.shape
    eidx, wts = _route(h, w_router, router_bias)
    tk = T * TOP_K
    e_flat = eidx.reshape(tk)
    tok_flat = jnp.repeat(jnp.arange(T, dtype=jnp.int32), TOP_K)
    w_flat = wts.reshape(tk)
    order = jnp.argsort(e_flat)
    e_sorted = e_flat[order]
    counts = jnp.zeros((N_EXPERTS,), jnp.int32).at[e_flat].add(1)
    starts = jnp.cumsum(counts) - counts
    padded = (counts + MOE_BLOCK - 1) // MOE_BLOCK * MOE_BLOCK
    pends = jnp.cumsum(padded)
    pstarts = pends - padded
    dest = pstarts[e_sorted] + jnp.arange(tk, dtype=jnp.int32) - starts[e_sorted]
    n_blocks = -(-tk // MOE_BLOCK) + N_EXPERTS
    n_rows = n_blocks * MOE_BLOCK
    tok_buf = jnp.full((n_rows,), T, jnp.int32).at[dest].set(tok_flat[order])
    gate_buf = jnp.zeros((n_rows,), h.dtype).at[dest].set(w_flat[order].astype(h.dtype))
    block_start = jnp.arange(n_blocks, dtype=jnp.int32) * MOE_BLOCK
    block_e = jnp.minimum(jnp.searchsorted(pends, block_start, side="right"), N_EXPERTS - 1)
    h_pad = jnp.concatenate([h, jnp.zeros((1, D), h.dtype)], axis=0)

    def body(acc, blk):
        tok, gate, e = blk
        rows = h_pad[tok]
        a = jax.nn.silu(rows @ w_eg[e]) * (rows @ w_eu[e])
        y = (a @ w_ed[e]) * gate[:, None]
        return acc.at[tok].add(y), None

    acc, _ = lax.scan(body, jnp.zeros((T + 1, D), h.dtype),
                      (tok_buf.reshape(n_blocks, MOE_BLOCK), gate_buf.reshape(n_blocks, MOE_BLOCK), block_e))
    shared = (jax.nn.silu(h @ w_sg) * (h @ w_su)) @ w_sd
    return acc[:T] + shared


def setup_inputs(seed: int = 0) -> dict:
    key = jax.random.key(seed)
    ks = jax.random.split(key, 27)
    f32 = jnp.float32
    L = DEPTH
    D = D_MODEL

    def nrm(k, shape, scale):
        return jax.random.normal(k, shape, f32) * scale

    def gain(k, n):
        return 1.0 + 0.05 * jax.random.normal(k, (L, n), f32)

    return {
        "x": nrm(ks[0], (BATCH, SEQ, D), 1.0),
        "c": nrm(ks[1], (BATCH, D), 1.0),
        "ctx": nrm(ks[2], (BATCH, CTX_LEN, D), 1.0),
        "c_ctx": nrm(ks[3], (D,), 1.0),
        "w_mod": nrm(ks[4], (L, D, 6 * D), 0.5 * D ** -0.5),
        "b_mod": nrm(ks[5], (L, 6 * D), 0.02),
        "norm_mix_pre": gain(ks[6], D),
        "norm_mix_post": gain(ks[7], D),
        "norm_ffn_pre": gain(ks[8], D),
        "norm_ffn_post": gain(ks[9], D),
        "w_in": nrm(ks[10], (L, D, D_IN), D ** -0.5),
        "w_a2_fwd": nrm(ks[11], (L, GLA_RANK, GLA_KEY_WIDTH), GLA_RANK ** -0.5),
        "b_a_fwd": nrm(ks[12], (L, GLA_KEY_WIDTH), 0.5),
        "w_a2_bwd": nrm(ks[13], (L, GLA_RANK, GLA_KEY_WIDTH), GLA_RANK ** -0.5),
        "b_a_bwd": nrm(ks[14], (L, GLA_KEY_WIDTH), 0.5),
        "gla_norm": gain(ks[15], GLA_WIDTH),
        "w_pool": nrm(ks[16], (L, len(POOL_WINDOWS), POOL_GROUP, POOL_GROUP), POOL_GROUP ** -0.5),
        "pool_scale": gain(ks[17], POOL_WIDTH),
        "w_out": nrm(ks[18], (L, D_MIX, D), D_MIX ** -0.5),
        "w_router": nrm(ks[19], (L, D, N_EXPERTS), D ** -0.5),
        "router_bias": nrm(ks[20], (L, N_EXPERTS), 0.01),
        "w_exp_gate": nrm(ks[21], (L, N_EXPERTS, D, D_EXPERT), D ** -0.5),
        "w_exp_up": nrm(ks[22], (L, N_EXPERTS, D, D_EXPERT), D ** -0.5),
        "w_exp_down": nrm(ks[23], (L, N_EXPERTS, D_EXPERT, D), D_EXPERT ** -0.5),
        "w_sh_gate": nrm(ks[24], (L, D, D_SHARED), D ** -0.5),
        "w_sh_up": nrm(ks[25], (L, D, D_SHARED), D ** -0.5),
        "w_sh_down": nrm(ks[26], (L, D_SHARED, D), D_SHARED ** -0.5),
    }


def reference(x, c, ctx, c_ctx, w_mod, b_mod, norm_mix_pre, norm_mix_post, norm_ffn_pre,
              norm_ffn_post, w_in, w_a2_fwd, b_a_fwd, w_a2_bwd, b_a_bwd, gla_norm, w_pool,
              pool_scale, w_out, w_router, router_bias, w_exp_gate, w_exp_up, w_exp_down,
              w_sh_gate, w_sh_up, w_sh_down):
    B = x.shape[0]
    zero_state = jnp.zeros((B, GLA_HEADS, GLA_DK, GLA_DV), jnp.float32)
    for i in range(DEPTH):
        last = i == DEPTH - 1
        mod = jax.nn.silu(c) @ w_mod[i] + b_mod[i]
        mod_c = jax.nn.silu(c_ctx) @ w_mod[i] + b_mod[i]
        sh1, sc1, gt1, sh2, sc2, gt2 = jnp.split(mod[:, None, :], 6, axis=-1)
        csh1, csc1, cgt1, csh2, csc2, cgt2 = jnp.split(mod_c, 6, axis=-1)
        mix_w = (w_a2_fwd[i], b_a_fwd[i], w_a2_bwd[i], b_a_bwd[i], gla_norm[i], w_pool[i],
                 pool_scale[i], w_out[i])
        ffn_w = (w_router[i], router_bias[i], w_exp_gate[i], w_exp_up[i], w_exp_down[i],
                 w_sh_gate[i], w_sh_up[i], w_sh_down[i])
        hc = _rmsnorm(ctx, norm_mix_pre[i]) * (1 + csc1) + csh1
        y_c, s_f, s_b = _token_mixer(hc @ w_in[i], zero_state, zero_state, *mix_w,
                                     grid=False, with_output=not last)
        h = _rmsnorm(x, norm_mix_pre[i]) * (1 + sc1) + sh1
        y, _, _ = _token_mixer(h @ w_in[i], s_f, s_b, *mix_w, grid=True, with_output=True)
        x = x + gt1 * _rmsnorm(y, norm_mix_post[i])
        h = _rmsnorm(x, norm_ffn_pre[i]) * (1 + sc2) + sh2
        y = _moe(h.reshape(-1, D_MODEL), *ffn_w).reshape(x.shape)
        x = x + gt2 * _rmsnorm(y, norm_ffn_post[i])
        if not last:
            ctx = ctx + cgt1 * _rmsnorm(y_c, norm_mix_post[i])
            hc = _rmsnorm(ctx, norm_ffn_pre[i]) * (1 + csc2) + csh2
            y_c = _moe(hc.reshape(-1, D_MODEL), *ffn_w).reshape(ctx.shape)
            ctx = ctx + cgt2 * _rmsnorm(y_c, norm_ffn_post[i])
    return x
```

```python
import os
from contextlib import ExitStack
import numpy as np
import ml_dtypes
import concourse.bass as bass
import concourse.mybir as mybir
from concourse.bass_utils import run_bass_kernel_spmd

F32 = mybir.dt.float32
BF16 = mybir.dt.bfloat16
U32 = mybir.dt.uint32
ALU = mybir.AluOpType
AF = mybir.ActivationFunctionType

P = 128
D = 2048
KD = 16
NT = 16
NTT = 18
NE = 64
CAPT = [7] + [6] * 2 + [5] * 4 + [4] * 18 + [3] * 20 + [2] * 19
assert len(CAPT) == NE
TSTART = [sum(CAPT[:i]) for i in range(NE)]
NTOT = sum(CAPT)
NTE = max(CAPT)
CAP = NTE * 128
DUMP = NTOT * 128
NROWS = NTOT * 128 + 128
EPS = 1e-6
QSCALE = 128 ** -0.5
WINS = (2, 4, 8, 16)
SCK = int(os.environ.get("SCK", "2"))

def _pool_deltas(w):
    lo, hi = -(w // 2), w // 2 - 1
    ds = []
    for d in range(-8, 9):
        ok = any(lo <= 2 * d + a - b <= hi for a in (0, 1) for b in (0, 1))
        if ok:
            ds.append(d)
    return ds

POOL_IDX = {}
_n = 0
for _w in WINS:
    for _d in _pool_deltas(_w):
        POOL_IDX[(_w, _d)] = _n
        _n += 1
NPOOLM = _n

CF = {}
_o = 0
for _name, _wd in (("TQf", 256), ("TQb", 256), ("SLf", 128), ("SLb", 128), ("MF", 512), ("MB", 512),
                   ("ones", 1), ("invcnt", 64), ("iotar", 64), ("tbrow", 64), ("iotac", 1), ("tbcol", 1),
                   ("kqp", 4), ("ones64", 64)):
    CF[_name] = (_o, _o + _wd)
    _o += _wd
NCF = _o
CB = {"ident": (0, 128), "tri": (128, 256), "ones": (256, 384), "pool": (384, 384 + 128 * NPOOLM)}
NCB0 = 384 + 128 * NPOOLM
NCB = NCB0 + 3 * 128


def _make_consts():
    j = np.arange(128)[:, None]
    i = np.arange(128)[None, :]
    mid = 64
    cf = np.zeros((128, NCF), np.float32)
    le = (j <= i).astype(np.float32)
    ge = (j >= i).astype(np.float32)
    cf[:, CF["TQf"][0]:CF["TQf"][0] + 128] = le
    cf[:, CF["TQf"][0] + 128:CF["TQf"][1]] = le - (j <= mid).astype(np.float32)
    cf[:, CF["TQb"][0]:CF["TQb"][0] + 128] = ge
    cf[:, CF["TQb"][0] + 128:CF["TQb"][1]] = ge - (j >= mid).astype(np.float32)
    cf[:, CF["SLf"][0]:CF["SLf"][1]] = (j > i)
    cf[:, CF["SLb"][0]:CF["SLb"][1]] = (j < i)
    cf[:, CF["MF"][0]:CF["MF"][1]] = np.tile(le, (1, 4))
    cf[:, CF["MB"][0]:CF["MB"][1]] = np.tile(ge, (1, 4))
    cf[:, CF["ones"][0]] = 1.0
    rows = 32
    def cnt(pos, w, n):
        return np.minimum(pos + w // 2, n) - np.maximum(pos - w // 2, 0)
    ic = np.zeros((128, 64), np.float32)
    t = np.arange(128)
    for to in range(16):
        r = 2 * to + t // 64
        c = t % 64
        for g, w in enumerate(WINS):
            ic[:, to * 4 + g] = 1.0 / (cnt(r, w, rows) * cnt(c, w, 64))
    cf[:, CF["invcnt"][0]:CF["invcnt"][1]] = ic
    cf[:, CF["iotar"][0]:CF["iotar"][1]] = np.arange(64)[None, :]
    cf[:, CF["tbrow"][0]:CF["tbrow"][1]] = (63 - np.arange(64))[None, :]
    cf[:, CF["iotac"][0]] = np.arange(128)
    cf[:, CF["tbcol"][0]] = 63 - np.arange(128)
    cf[:, CF["kqp"][0]:CF["kqp"][1]] = np.arange(4)[None, :] * 128 + np.arange(128)[:, None]
    cf[:, CF["ones64"][0]:CF["ones64"][1]] = 1.0
    cb = np.zeros((128, NCB), np.float32)
    cb[:, 0:128] = np.eye(128)
    cb[:, 128:256] = (j < i)
    cb[:, 256:384] = 1.0
    a_in = (np.arange(128) // 64)[:, None]
    c_in = (np.arange(128) % 64)[:, None]
    b_out = (np.arange(128) // 64)[None, :]
    c_out = (np.arange(128) % 64)[None, :]
    for (w, d), n in POOL_IDX.items():
        lo, hi = -(w // 2), w // 2 - 1
        dr = 2 * d + a_in - b_out
        dc = c_in - c_out
        m = ((dr >= lo) & (dr <= hi) & (dc >= lo) & (dc <= hi)).astype(np.float32)
        cb[:, 384 + n * 128:384 + (n + 1) * 128] = m
    pidx = np.arange(128)
    cb[:, NCB0:NCB0 + 128] = pidx[:, None]
    cb[:64, NCB0 + 128:NCB0 + 256] = np.array(TSTART, np.float32)[:, None]
    cb[:64, NCB0 + 256:NCB0 + 384] = np.array(CAPT, np.float32)[:, None]
    return cf, cb.astype(ml_dtypes.bfloat16)


class Tok:
    __slots__ = ("sem", "val", "own")

    def __init__(self, sem=None, val=None, own=None):
        self.sem, self.val, self.own = sem, val, own


class Sched:
    ENGS = ("pe", "act", "dve", "pool", "sp")
    LIMIT = 12000

    def __init__(self, nc, stack):
        self.nc, self.stack = nc, stack
        self.ops = {e: [] for e in self.ENGS}
        self.esem = {e: self._newsem("e_" + e) for e in self.ENGS}
        self.ecnt = {e: 0 for e in self.ENGS}
        self.pend = {e: None for e in self.ENGS}
        self.lastw, self.readers = {}, {}
        self.dsem, self.dcnt, self.dlast = {}, {}, {}
        self.all_toks = {}
        self.fence_deps = {e: [] for e in self.ENGS}
        self.nsem = 5

    def _newsem(self, name):
        return self.stack.enter_context(self.nc.semaphore(name))

    def _deps(self, reads, writes):
        deps = []
        for r in reads:
            t = self.lastw.get(r)
            if t is not None:
                deps.append(t)
        for w in writes:
            t = self.lastw.get(w)
            if t is not None:
                deps.append(t)
            deps.extend(self.readers.get(w, ()))
        return deps

    def _commit(self, tok, reads, writes):
        for r in reads:
            self.readers.setdefault(r, []).append(tok)
        for w in writes:
            self.lastw[w] = tok
            self.readers[w] = []

    def op(self, eng, fn, reads=(), writes=(), inc=True):
        deps = self._deps(reads, writes) + self.fence_deps[eng]
        self.fence_deps[eng] = []
        if inc:
            if self.ecnt[eng] >= self.LIMIT:
                self.esem[eng] = self._newsem("e_%s_%d" % (eng, self.nsem))
                self.nsem += 1
                self.ecnt[eng] = 0
            self.ecnt[eng] += 1
            tok = self.pend[eng]
            if tok is None:
                tok = Tok(own=eng)
            tok.sem, tok.val = self.esem[eng], self.ecnt[eng]
            self.pend[eng] = None
            self.all_toks[id(tok.sem)] = tok
        else:
            tok = self.pend[eng]
            if tok is None:
                tok = Tok(own=eng)
                self.pend[eng] = tok
        self.ops[eng].append((deps, fn, "c" if inc else "n", tok))
        self._commit(tok, reads, writes)
        return tok

    def dma(self, eng, fn, key, reads=(), writes=(), n=1):
        if key not in self.dsem:
            self.dsem[key] = self._newsem("d_" + str(key))
            self.nsem += 1
            self.dcnt[key] = 0
        deps = self._deps(reads, writes) + self.fence_deps[eng]
        self.fence_deps[eng] = []
        if key in self.dlast:
            deps.append(self.dlast[key])
        self.dcnt[key] += 16 * n
        tok = Tok(self.dsem[key], self.dcnt[key], own="dma")
        self.dlast[key] = tok
        self.all_toks[id(tok.sem)] = tok
        self.ops[eng].append((deps, fn, "d", tok))
        self._commit(tok, reads, writes)
        return tok

    def fence(self):
        toks = list(self.all_toks.values())
        for e in self.ENGS:
            self.fence_deps[e] = list(toks)
        self.lastw, self.readers = {}, {}

    def replay(self, block, final=False):
        nc = self.nc
        ops, self.ops = self.ops, {e: [] for e in self.ENGS}
        for e in self.ENGS:
            assert self.pend[e] is None, "unfinished group on " + e
        tail = list(self.all_toks.values()) if final else []

        def run(e, eng):
            known = self.known.setdefault(e, {})
            for deps, fn, kind, tok in ops[e]:
                for d in deps:
                    if d is tok:
                        continue
                    if e == "pe" and d.own == "pe":
                        continue
                    if known.get(id(d.sem), 0) >= d.val:
                        continue
                    eng.wait_ge(d.sem, d.val)
                    known[id(d.sem)] = d.val
                r = fn(eng)
                if kind == "c":
                    r.then_inc(tok.sem, 1)
                elif kind == "d":
                    if not isinstance(r, (list, tuple)):
                        r = [r]
                    for ins in r:
                        ins.then_inc(tok.sem, 16)
            if e == "sp":
                for d in tail:
                    if known.get(id(d.sem), 0) >= d.val:
                        continue
                    eng.wait_ge(d.sem, d.val)
                    known[id(d.sem)] = d.val

        @block.sync
        def _(eng):
            run("sp", eng)

        @block.scalar
        def _(eng):
            run("act", eng)

        @block.vector
        def _(eng):
            run("dve", eng)

        @block.tensor
        def _(eng):
            run("pe", eng)

        @block.gpsimd
        def _(eng):
            run("pool", eng)

    known = None


def build(stop=99, dbg=(), sub=9):
    nc = bass.Bass("TRN2", target_bir_lowering=False)

    def inp(name, shape, dt=F32):
        return nc.dram_tensor(name, list(shape), dt, kind="ExternalInput").ap()

    def scratch(name, shape, dt):
        kind = "ExternalOutput" if name in dbg else "Internal"
        return nc.dram_tensor(name, list(shape), dt, kind=kind).ap()

    x = inp("x", [2048, D])
    ctx = inp("ctx", [256, D])
    cc = inp("cc", [P, 32])
    w_mod = inp("w_mod", [D, 6 * D])
    b_mod = inp("b_mod", [1, 6 * D])
    nrm = inp("nrm", [4, D])
    w_in = inp("w_in", [D, 4128])
    wa2 = inp("wa2", [33, 1024])
    gla_norm = inp("gla_norm", [1, 1024])
    w_pool = inp("w_pool", [4, 256, 256])
    pool_scale = inp("pool_scale", [1, 1024])
    w_out = inp("w_out", [D, D])
    w_router = inp("w_router", [D, NE])
    router_bias = inp("router_bias", [1, NE])
    if stop >= 5:
        w_eg = inp("w_eg", [NE * 512, D])
        w_eu = inp("w_eu", [NE * 512, D])
        w_ed = inp("w_ed", [NE * 512, D])
    w_sg = inp("w_sg", [D, 512])
    w_su = inp("w_su", [D, 512])
    w_sd = inp("w_sd", [512, D])
    cf_in = inp("cf", [P, NCF])
    cb_in = inp("cb", [P, NCB], BF16)
    out = nc.dram_tensor("out", [2048, D], F32, kind="ExternalOutput").ap()

    modD = scratch("modD", [2, 6 * D], F32)
    U_d = scratch("U_d", [NTT * P, 4096], BF16)
    G_d = scratch("G_d", [NTT * P, 1024], F32)
    Y_d = scratch("Y_d", [2048, D], BF16)
    X1_d = scratch("X1_d", [2048, D], F32)
    H_d = scratch("H_d", [2048, D], BF16)
    XG_d = scratch("XG_d", [NROWS, D], BF16)
    YS_d = scratch("YS_d", [2048, D], BF16)
    YG_d = scratch("YG_d", [NROWS, D], BF16)

    stack = ExitStack()
    with stack:
        S = Sched(nc, stack)
        S.known = {}

        def sb(name, shape, dt):
            return stack.enter_context(nc.sbuf_tensor("sb_" + name, list(shape), dt))

        def ps(name, shape, dt):
            return stack.enter_context(nc.psum_tensor("ps_" + name, list(shape), dt))

        cf = sb("cf", [P, NCF], F32)
        cb = sb("cb", [P, NCB], BF16)
        dsti = sb("dsti", [P, NT * 8], U32)
        widx = sb("widx", [P, NE * 4], U32)
        gate8 = sb("gate8", [P, NT * 8], F32)

        def CFs(name, a=None, b=None):
            o0, o1 = CF[name]
            if a is not None:
                o0, o1 = o0 + a, o0 + b
            return cf[:, o0:o1]

        ident = cb[:, 0:128]
        tri_b = cb[:, 128:256]
        ones_b = cb[:, 256:384]

        def poolm(n):
            return cb[:, 384 + n * 128:384 + (n + 1) * 128]

        psT = [ps("psT%d" % i, [P, 1024], BF16) for i in range(2)]
        psM = [ps("psM%d" % i, [P, 512], F32) for i in range(6)]
        RT = [("psT", i) for i in range(2)]
        RM = [("psM", i) for i in range(6)]

        nc_rt = [None]
        S.dma("sp", lambda e: e.dma_start(out=cf[:, :], in_=cf_in), "c0", writes=["cf"])
        S.dma("sp", lambda e: e.dma_start(out=cb[:, :], in_=cb_in), "c1", writes=["cb"])

        rr = {"ev": 0}

        def evac_eng():
            rr["ev"] ^= 1
            return "act" if rr["ev"] else "dve"

        def copy_op(eng, out_ap, in_ap, reads, writes):
            if eng == "act":
                S.op("act", lambda e: e.copy(out=out_ap, in_=in_ap), reads, writes)
            elif eng == "dve":
                S.op("dve", lambda e: e.tensor_copy(out=out_ap, in_=in_ap), reads, writes)
            else:
                S.op("pool", lambda e: e.tensor_copy(out=out_ap, in_=in_ap), reads, writes)

        def bcast_load(dst, src_row, key, wname):
            wn = wname if isinstance(wname, list) else [wname]
            S.dma("sp", lambda e: e.dma_start(out=dst, in_=src_row.partition_broadcast(P)), key, writes=wn)

        def rms_rstd(src_ap, n, junk, ssq, rstd, reads, tag, junk_res=None):
            S.op("act", lambda e: e.activation(out=junk, in_=src_ap, func=AF.Square, accum_out=ssq),
                 reads=reads, writes=[junk_res if junk_res is not None else "junk" + tag, "ssq" + tag])
            S.op("act", lambda e: e.activation(out=ssq, in_=ssq, func=AF.Sqrt, bias=epsb[:, :], scale=1.0 / n),
                 reads=["ssq" + tag, "epsb"], writes=["ssq" + tag])
            S.op("dve", lambda e: e.reciprocal(out=rstd, in_=ssq),
                 reads=["ssq" + tag], writes=["rstd" + tag])

        epsb = sb("epsb", [P, 1], F32)
        ccs = sb("ccs", [P, 32], F32)
        ccb = sb("ccb", [P, 32], BF16)
        ccb3 = ccb[:, :].rearrange("p (k t) -> p k t", t=2)

        def mod_group(n, wbufs, bmt_, mods_, pm, rpm, tag, do_load=True, do_comp=True):
            s = n % len(wbufs)
            if do_load:
              S.dma("pool", lambda e: e.dma_start(
                out=wbufs[s][:, :, :], in_=w_mod[:, n * 512:(n + 1) * 512].rearrange("(k p) n -> p k n", p=P)),
                "wst%d" % s, writes=[("wst" + tag, s)])
            if not do_comp:
                return
            S.dma("sp", lambda e: e.dma_start(
                out=bmt_[:, :], in_=b_mod[0:1, n * 512:(n + 1) * 512].partition_broadcast(2)),
                "bm0", writes=["bmt" + tag])
            for k in range(KD):
                S.op("pe", (lambda k=k: lambda e: e.matmul(
                    pm[0:2, :], lhsT=ccb3[:, k, :], rhs=wbufs[s][:, k, :], start=(k == 0), stop=(k == KD - 1)))(),
                    reads=["ccb", ("wst" + tag, s)], writes=[rpm], inc=(k == KD - 1))
            S.op("dve", lambda e: e.tensor_tensor(out=mods_[:, :], in0=pm[0:2, :], in1=bmt_[:, :], op=ALU.add),
                 reads=[rpm, "bmt" + tag], writes=["mods" + tag])
            S.dma("sp", lambda e: e.dma_start(out=modD[:, n * 512:(n + 1) * 512], in_=mods_[:, :]),
                  "mo0", reads=["mods" + tag], writes=[("modD", n // 4)])
        S.op("dve", lambda e: e.memset(epsb[:, :], EPS), writes=["epsb"])

        def transposes(src_fn, nblk, dst_fn, reads, wname_fn, group=4):
            for g0 in range(0, nblk, group):
                n = min(group, nblk - g0)
                bank = (g0 // group) % 2
                base = 0
                for ii in range(n):
                    i = g0 + ii
                    o = psT[bank][:, base + ii * 128:base + (ii + 1) * 128]
                    S.op("pe", (lambda o=o, i=i: lambda e: e.transpose(out=o, in_=src_fn(i), identity=ident))(),
                         reads=list(reads) + ["cb"], writes=[("psT", bank)], inc=(ii == n - 1))
                src = psT[bank][:, base:base + n * 128].rearrange("p (a b) -> p a b", b=128)
                copy_op(evac_eng(), dst_fn(g0, n), src, reads=[("psT", bank)], writes=[wname_fn(g0)])

        es1 = ExitStack()
        with es1:
            def sb1(name, shape, dt):
                return es1.enter_context(nc.sbuf_tensor("e1_" + name, list(shape), dt))
            wst = [sb1("wst%d" % i, [P, KD, 512], BF16) for i in range(2)]
            bmt = [sb1("bmt0", [2, 512], F32)] * 2
            mods = [sb1("mods0", [2, 512], F32)] * 2
            gm1 = sb1("gm1", [P, D], F32)
            sh1 = sb1("sh1", [P, D], F32)
            cgm1 = sb1("cgm1", [P, D], F32)
            csh1 = sb1("csh1", [P, D], F32)
            xt = [sb1("xt%d" % i, [P, D], F32) for i in range(2)]
            hb = [sb1("hb%d" % i, [P, D], BF16) for i in range(2)]
            ssq = sb1("ssq", [P, 1], F32)
            rstd = sb1("rstd", [P, 1], F32)
            hT = sb1("hT", [P, KD, NTT * P], BF16)
            ust = [sb1("ust%d" % i, [P, 512], BF16) for i in range(2)]
            ab = sb1("ab", [P, 32], BF16)
            aT = sb1("aT", [33, 2 * P], BF16)
            wa_in = sb1("wa_in", [P, KD, 32], BF16)
            wa2b = sb1("wa2b", [33, 1024], BF16)
            ez = sb1("ez", [P, 1024], F32)
            gst = [ez, ez]
            wmx = sb1("wmx", [P, KD, 512], BF16)

            S.dma("sp", lambda e: e.dma_start(out=ccs[:, :], in_=cc), "c2", writes=["ccs"])
            S.op("act", lambda e: e.activation(out=ccb[:, :], in_=ccs[:, :], func=AF.Silu), reads=["ccs"], writes=["ccb"])
            for n in range(8):
                mod_group(n, wst, bmt[0], mods[0], psM[n % 2], RM[n % 2], "1")

            if sub <= 1:
                with nc.Block() as block:
                    S.replay(block, final=True)
                return nc
            def modrow(r, c):
                return modD[r:r + 1, c * D:(c + 1) * D]
            tmpA = xt[0]
            bcast_load(tmpA[:, :], nrm[0:1, :], "b0", ("xt", 0))
            def mk_gm(dst, dname, row, key):
                S.dma("sp", lambda e: e.dma_start(out=dst[:, :], in_=modrow(row, 1).partition_broadcast(P)), key,
                      reads=[("modD", 1)], writes=[dname])
                S.op("dve", lambda e: e.scalar_tensor_tensor(out=dst[:, :], in0=dst[:, :], scalar=1.0, in1=tmpA[:, :],
                                                             op0=ALU.add, op1=ALU.mult),
                     reads=[dname, ("xt", 0)], writes=[dname])
            mk_gm(gm1, "gm1", 0, "b1")
            mk_gm(cgm1, "cgm1", 1, "b2")
            S.dma("sp", lambda e: e.dma_start(out=sh1[:, :], in_=modrow(0, 0).partition_broadcast(P)), "b3",
                  reads=[("modD", 0)], writes=["sh1"])
            S.dma("sp", lambda e: e.dma_start(out=csh1[:, :], in_=modrow(1, 0).partition_broadcast(P)), "b4",
                  reads=[("modD", 0)], writes=["csh1"])

            S.dma("pool", lambda e: e.dma_start(out=wa2b[:, :], in_=wa2), "c3", writes=["wa2b"])
            S.op("pool", lambda e: e.memset(aT[32:33, :], 1.0), writes=["aT1"])
            S.dma("pool", lambda e: e.dma_start(out=wa_in[:, :, :], in_=w_in[:, 3072:3104].rearrange("(k p) n -> p k n", p=P)),
                  "c3", writes=["wa_in"])
            hT_reads = lambda tt: [("hT", tt, g0) for g0 in range(0, KD, 4)]
            groups = [(0, 512, 0), (512, 512, 512), (1024, 512, 1024), (1536, 512, 1536),
                      (2048, 512, 2048), (2560, 512, 2560), (3104, 512, 3072), (3616, 512, 3584)]
            mmr = [0]
            def win_load(gi):
                c0, ncol, u0 = groups[gi]
                s = gi % 2
                S.dma("pool", lambda e: e.dma_start(
                    out=wst[s][:, :, 0:ncol], in_=w_in[:, c0:c0 + ncol].rearrange("(k p) n -> p k n", p=P)),
                    "wst%d" % s, writes=[("wst1", s)])

            def win_block(gi, tt):
                c0, ncol, u0 = groups[gi]
                s = gi % 2
                if True:
                    pm = psM[2 + mmr[0] % 4]
                    rpm = RM[2 + mmr[0] % 4]
                    mmr[0] += 1
                    for k in range(KD):
                        S.op("pe", (lambda k=k, s=s, pm=pm, tt=tt, ncol=ncol: lambda e: e.matmul(
                            pm[:, 0:ncol], lhsT=hT[:, k, tt * P:(tt + 1) * P], rhs=wst[s][:, k, 0:ncol],
                            start=(k == 0), stop=(k == KD - 1)))(),
                            reads=hT_reads(tt) + [("wst1", s)], writes=[rpm], inc=(k == KD - 1))
                    if u0 is not None:
                        us = mmr[0] % 2
                        copy_op(evac_eng(), ust[us][:, :], pm[:, :], reads=[rpm], writes=[("ust", us)])
                        S.dma("sp", (lambda us=us, tt=tt, u0=u0: lambda e: e.dma_start(
                            out=U_d[tt * P:(tt + 1) * P, u0:u0 + 512], in_=ust[us][:, :]))(),
                            "us%d" % us, reads=[("ust", us)], writes=[("U_d", tt)])

            def a1(tt):
                pm = psM[2 + mmr[0] % 4]
                rpm = RM[2 + mmr[0] % 4]
                mmr[0] += 1
                for k in range(KD):
                    S.op("pe", (lambda k=k: lambda e: e.matmul(
                        pm[:, 0:32], lhsT=hT[:, k, tt * P:(tt + 1) * P], rhs=wa_in[:, k, :],
                        start=(k == 0), stop=(k == KD - 1)))(),
                        reads=hT_reads(tt) + ["wa_in"], writes=[rpm], inc=(k == KD - 1))
                S.op("act", lambda e: e.copy(out=ab[:, :], in_=pm[:, 0:32]), reads=[rpm], writes=["ab"])

            def a2(tt):
                sl = tt % 2
                S.op("pe", lambda e: e.transpose(out=psT[0][0:32, 0:128], in_=ab[:, :], identity=ident),
                     reads=["ab", "cb"], writes=[("psT", 0)])
                S.op("dve", lambda e: e.tensor_copy(out=aT[0:32, sl * P:(sl + 1) * P], in_=psT[0][0:32, 0:128]),
                     reads=[("psT", 0)], writes=[("aT", sl)])

            def a3(tt):
                sl = tt % 2
                for hh in range(2):
                    S.op("pe", (lambda hh=hh: lambda e: e.matmul(
                        psM[hh][:, :], lhsT=aT[:, sl * P:(sl + 1) * P], rhs=wa2b[:, hh * 512:(hh + 1) * 512], start=True, stop=True))(),
                        reads=[("aT", sl), "aT1", "wa2b"], writes=[RM[hh]])
                    S.op("act", (lambda hh=hh: lambda e: e.activation(
                        out=ez[:, hh * 512:(hh + 1) * 512], in_=psM[hh][:, :], func=AF.Exp, scale=-1.0))(),
                        reads=[RM[hh]], writes=[("ez", hh)])
                    S.op("act", (lambda hh=hh: lambda e: e.activation(
                        out=ez[:, hh * 512:(hh + 1) * 512], in_=ez[:, hh * 512:(hh + 1) * 512], func=AF.Ln,
                        bias=1.0, scale=1.0))(),
                        reads=[("ez", hh)], writes=[("ez", hh)])
                S.op("dve", lambda e: e.tensor_scalar(
                    out=ez[:, :], in0=ez[:, :], scalar1=-1.0 / 16.0, scalar2=None, op0=ALU.mult),
                    reads=[("ez", 0), ("ez", 1)], writes=[("ez", 0), ("ez", 1)])
                S.dma("sp", lambda e: e.dma_start(out=G_d[tt * P:(tt + 1) * P, :], in_=ez[:, :]),
                      "gs0", reads=[("ez", 0), ("ez", 1)], writes=[("G_d", tt)])

            win_load(0)
            win_load(1)
            def h_load(tt):
                s = tt % 2
                src = ctx[tt * P:(tt + 1) * P, :] if tt < 2 else x[(tt - 2) * P:(tt - 1) * P, :]
                S.dma("sp", lambda e: e.dma_start(out=xt[s][:, :], in_=src), "xt%d" % s, writes=[("xt", s)])

            h_load(0)
            for tt in range(NTT):
                s = tt % 2
                if tt + 1 < NTT:
                    h_load(tt + 1)
                if tt >= 1 and sub > 2:
                    win_block(0, tt - 1)
                rms_rstd(xt[s][:, :], D, hb[s][:, :], ssq[:, :], rstd[:, :], [("xt", s)], "1", junk_res=("hb", s))
                g_, s_, gn, sn = (cgm1, csh1, "cgm1", "csh1") if tt < 2 else (gm1, sh1, "gm1", "sh1")
                S.op("dve", (lambda s=s, g_=g_: lambda e: e.scalar_tensor_tensor(
                    out=xt[s][:, :], in0=xt[s][:, :], scalar=rstd[:, 0:1], in1=g_[:, :], op0=ALU.mult, op1=ALU.mult))(),
                    reads=[("xt", s), "rstd1", gn], writes=[("xt", s)])
                S.op("dve", (lambda s=s, s_=s_: lambda e: e.tensor_tensor(
                    out=hb[s][:, :], in0=xt[s][:, :], in1=s_[:, :], op=ALU.add))(),
                    reads=[("xt", s), sn], writes=[("hb", s)])
                transposes(lambda i, s=s: hb[s][:, i * 128:(i + 1) * 128], KD,
                           lambda g0, n, tt=tt: hT[:, g0:g0 + n, tt * P:(tt + 1) * P],
                           [("hb", s)], lambda g0, tt=tt: ("hT", tt, g0))

            if sub <= 2:
                with nc.Block() as block:
                    S.replay(block, final=True)
                return nc
            win_block(0, NTT - 1)
            nb = 0
            mg = 8
            for gi in range(1, len(groups)):
                if gi + 1 < len(groups):
                    win_load(gi + 1)
                for tt in range(NTT):
                    if gi == len(groups) - 1:
                        a1(tt)
                    win_block(gi, tt)
                    if gi == len(groups) - 1:
                        a2(tt)
                        if tt >= 1:
                            a3(tt - 1)
                    nb += 1
                    if nb % 8 == 0 and mg < 24:
                        mod_group(mg, [wmx], bmt[0], mods[0], psM[mg % 2], RM[mg % 2], "x")
                        mg += 1
            a3(NTT - 1)
            while mg < 24:
                mod_group(mg, [wmx], bmt[0], mods[0], psM[mg % 2], RM[mg % 2], "x")
                mg += 1
            with nc.Block() as block:
                S.replay(block, final=(stop <= 1))
        if stop <= 1:
            return nc
        S.fence()

        es2 = ExitStack()
        with es2:
            def sb2(name, shape, dt):
                return es2.enter_context(nc.sbuf_tensor("e2_" + name, list(shape), dt))
            S32 = sb2("S32", [P, 8, 256], F32)
            Sbf = sb2("Sbf", [P, 8, 256], BF16)
            SbSt = sb2("SbSt", [P, NT, 1024], BF16)
            qk = [sb2("qk%d" % i, [P, 3072], BF16) for i in range(2)]
            gg = [sb2("gg%d" % i, [P, 1024], F32) for i in range(2)]
            ehat = sb2("ehat", [P, 512], F32)
            khat = sb2("khat", [P, 512], BF16)
            dec = sb2("dec", [P, 4], F32)
            eG = [sb2("eG%d" % i, [P, 512], F32) for i in range(2)]
            eq = [sb2("eq%d" % i, [P, 512], F32) for i in range(2)]
            ek = [sb2("ek%d" % i, [P, 512], F32) for i in range(2)]
            qh = [sb2("qh%d" % i, [P, 512], BF16) for i in range(2)]
            qt = [sb2("qt%d" % i, [P, 512], BF16) for i in range(2)]
            kt = [sb2("kt%d" % i, [P, 512], BF16) for i in range(2)]
            t1 = sb2("t1", [P, 512], F32)
            t2 = sb2("t2", [P, 512], F32)
            ATb = sb2("ATb", [P, 512], BF16)
            sr = sb2("sr", [P, 1024], F32)
            gnb = sb2("gnb", [P, 1024], F32)
            ygl = [sb2("ygl%d" % i, [P, 1024], BF16) for i in range(2)]
            junk2 = sb2("junk2", [P, 1024], BF16)
            dq = sb2("dq", [P, 4], F32)
            rstd4 = sb2("rstd4", [P, 4], F32)

            S.op("dve", lambda e: e.memset(S32[:, :, :], 0.0), writes=["S32f", "S32b"])
            S.op("pool", lambda e: e.memset(Sbf[:, :, :], 0.0), writes=["Sbff", "Sbfb"])
            bcast_load(gnb[:, :], gla_norm[0:1, :], "b0", "gnb")

            def load_tile(tt, s):
                S.dma("sp", lambda e: e.dma_start(out=qk[s][:, :], in_=U_d[tt * P:(tt + 1) * P, 0:3072]),
                      "qk%d" % s, reads=[("U_d", tt)], writes=[("qk", s)])
                S.dma("sp", lambda e: e.dma_start(out=gg[s][:, :], in_=G_d[tt * P:(tt + 1) * P, :]),
                      "gg%d" % s, reads=[("G_d", tt)], writes=[("gg", s)])

            def state_step(di, s, store=None):
                dn = "fb"[di]
                SL = CFs("SLf") if di == 0 else CFs("SLb")
                g_dir = gg[s][:, di * 512:(di + 1) * 512]
                S.op("pe", lambda e: e.matmul(psM[5][:, :], lhsT=SL, rhs=g_dir, start=True, stop=True),
                     reads=["cf", ("gg", s)], writes=[RM[5]])
                S.op("act", lambda e: e.activation(out=ehat[:, :], in_=psM[5][:, :], func=AF.Exp),
                     reads=[RM[5]], writes=["ehat"])
                S.op("dve", lambda e: e.tensor_tensor(out=khat[:, :], in0=qk[s][:, 512:1024], in1=ehat[:, :], op=ALU.mult),
                     reads=["ehat", ("qk", s)], writes=["khat"])
                for h in range(4):
                    S.op("pe", (lambda h=h: lambda e: e.matmul(
                        psM[5][:, h:h + 1], lhsT=gg[s][:, di * 512 + h * 128:di * 512 + (h + 1) * 128],
                        rhs=CFs("ones"), start=True, stop=True))(),
                        reads=["cf", ("gg", s)], writes=[RM[5]], inc=(h == 3))
                S.op("act", lambda e: e.activation(out=dec[:, :], in_=psM[5][:, 0:4], func=AF.Exp),
                     reads=[RM[5]], writes=["dec"])
                for h in range(4):
                    S.op("pe", (lambda h=h: lambda e: e.matmul(
                        psM[3 + h // 2][:, (h % 2) * 256:(h % 2 + 1) * 256], lhsT=khat[:, h * 128:(h + 1) * 128],
                        rhs=qk[s][:, 1024 + h * 256:1024 + (h + 1) * 256], start=True, stop=True))(),
                        reads=["khat", ("qk", s)], writes=[RM[3 + h // 2]], inc=(h % 2 == 1))
                if store is not None:
                    S.op("pool", lambda e: e.tensor_copy(out=SbSt[:, store, :],
                                                        in_=Sbf[:, di * 4:(di + 1) * 4, :].rearrange("p a b -> p (a b)")),
                         reads=["Sbf" + dn], writes=[("SbSt", store)])
                for h in range(4):
                    S.op("dve", (lambda h=h: lambda e: e.scalar_tensor_tensor(
                        out=S32[:, di * 4 + h, :], in0=S32[:, di * 4 + h, :], scalar=dec[:, h:h + 1],
                        in1=psM[3 + h // 2][:, (h % 2) * 256:(h % 2 + 1) * 256], op0=ALU.mult, op1=ALU.add))(),
                        reads=["S32" + dn, "dec", RM[3 + h // 2]], writes=["S32" + dn])
                S.op("act", lambda e: e.copy(out=Sbf[:, di * 4:(di + 1) * 4, :], in_=S32[:, di * 4:(di + 1) * 4, :]),
                     reads=["S32" + dn], writes=["Sbf" + dn])

            def out_step(s, n):
                for i in range(8):
                    S.op("pe", (lambda i=i: lambda e: e.transpose(
                        out=psT[0][:, i * 128:(i + 1) * 128], in_=qk[s][:, i * 128:(i + 1) * 128], identity=ident))(),
                        reads=[("qk", s), "cb"], writes=[("psT", 0)], inc=(i == 7))
                for di in range(2):
                    TQ = CFs("TQf") if di == 0 else CFs("TQb")
                    for h in range(4):
                        S.op("pe", (lambda h=h, di=di, TQ=TQ: lambda e: e.matmul(
                            psM[h // 2][:, (h % 2) * 256:(h % 2 + 1) * 256],
                            lhsT=gg[s][:, di * 512 + h * 128:di * 512 + (h + 1) * 128], rhs=TQ, start=True, stop=True))(),
                            reads=["cf", ("gg", s)], writes=[RM[h // 2]], inc=(h % 2 == 1))
                    for half in range(2):
                        pv = psM[half][:, :].rearrange("p (a b) -> p a b", b=256)
                        o3 = (lambda half: lambda t: t[:, half * 256:(half + 1) * 256].rearrange("p (a b) -> p a b", b=128))(half)
                        S.op("act", (lambda pv=pv, o3=o3, di=di: lambda e: e.activation(
                            out=o3(eG[di]), in_=pv[:, :, 0:128], func=AF.Exp))(),
                            reads=[RM[half]], writes=[("eG", di, half)])
                        S.op("act", (lambda pv=pv, o3=o3, di=di: lambda e: e.activation(
                            out=o3(eq[di]), in_=pv[:, :, 128:256], func=AF.Exp))(),
                            reads=[RM[half]], writes=[("eq", di, half)])
                        S.op("act", (lambda pv=pv, o3=o3, di=di: lambda e: e.activation(
                            out=o3(ek[di]), in_=pv[:, :, 128:256], func=AF.Exp, scale=-1.0))(),
                            reads=[RM[half]], writes=[("ek", di, half)])
                    rd = lambda nm: [(nm, di, 0), (nm, di, 1), ("psT", 0)]
                    S.op("dve", (lambda di=di: lambda e: e.scalar_tensor_tensor(
                        out=qh[di][:, :], in0=psT[0][:, 0:512], scalar=QSCALE, in1=eG[di][:, :], op0=ALU.mult, op1=ALU.mult))(),
                        reads=rd("eG"), writes=[("qh", di)])
                    S.op("dve", (lambda di=di: lambda e: e.scalar_tensor_tensor(
                        out=qt[di][:, :], in0=psT[0][:, 0:512], scalar=QSCALE, in1=eq[di][:, :], op0=ALU.mult, op1=ALU.mult))(),
                        reads=rd("eq"), writes=[("qt", di)])
                    S.op("dve", (lambda di=di: lambda e: e.tensor_tensor(
                        out=kt[di][:, :], in0=psT[0][:, 512:1024], in1=ek[di][:, :], op=ALU.mult))(),
                        reads=rd("ek"), writes=[("kt", di)])
                    for h in range(4):
                        S.op("pe", (lambda h=h, di=di: lambda e: e.matmul(
                            psM[2 + di][:, h * 128:(h + 1) * 128], lhsT=kt[di][:, h * 128:(h + 1) * 128],
                            rhs=qt[di][:, h * 128:(h + 1) * 128], start=True, stop=True))(),
                            reads=[("kt", di), ("qt", di)], writes=[RM[2 + di]], inc=(h == 3))
                S.op("dve", lambda e: e.tensor_tensor(out=t1[:, :], in0=psM[2][:, :], in1=CFs("MF"), op=ALU.mult),
                     reads=[RM[2], "cf"], writes=["t1"])
                S.op("dve", lambda e: e.tensor_tensor(out=t2[:, :], in0=psM[3][:, :], in1=CFs("MB"), op=ALU.mult),
                     reads=[RM[3], "cf"], writes=["t2"])
                S.op("pool", lambda e: e.tensor_tensor(out=ATb[:, :], in0=t1[:, :], in1=t2[:, :], op=ALU.add),
                     reads=["t1", "t2"], writes=["ATb"])
                for h in range(4):
                    po = psM[4 + h // 2][:, (h % 2) * 256:(h % 2 + 1) * 256]
                    rpo = RM[4 + h // 2]
                    S.op("pe", (lambda h=h, po=po: lambda e: e.matmul(
                        po, lhsT=qh[0][:, h * 128:(h + 1) * 128], rhs=Sbf[:, h, :], start=True, stop=False))(),
                        reads=[("qh", 0), "Sbff"], writes=[rpo], inc=False)
                    S.op("pe", (lambda h=h, po=po: lambda e: e.matmul(
                        po, lhsT=qh[1][:, h * 128:(h + 1) * 128], rhs=SbSt[:, n, h * 256:(h + 1) * 256], start=False, stop=False))(),
                        reads=[("qh", 1), ("SbSt", n)], writes=[rpo], inc=False)
                    S.op("pe", (lambda h=h, po=po: lambda e: e.matmul(
                        po, lhsT=ATb[:, h * 128:(h + 1) * 128], rhs=qk[s][:, 1024 + h * 256:1024 + (h + 1) * 256],
                        start=False, stop=True))(),
                        reads=["ATb", ("qk", s)], writes=[rpo], inc=(h % 2 == 1))
                for h in range(4):
                    S.op("act", (lambda h=h: lambda e: e.activation(
                        out=junk2[:, h * 256:(h + 1) * 256], in_=psM[4 + h // 2][:, (h % 2) * 256:(h % 2 + 1) * 256],
                        func=AF.Square, accum_out=dq[:, h:h + 1]))(),
                        reads=[RM[4 + h // 2]], writes=[("junk2", h), ("dq", h)])
                S.op("act", lambda e: e.activation(out=dq[:, :], in_=dq[:, :], func=AF.Sqrt, bias=epsb[:, :], scale=1.0 / 256),
                     reads=[("dq", h) for h in range(4)] + ["epsb"], writes=[("dq", h) for h in range(4)])
                S.op("dve", lambda e: e.reciprocal(out=rstd4[:, :], in_=dq[:, :]),
                     reads=[("dq", h) for h in range(4)], writes=["rstd4"])
                S.op("act", lambda e: e.activation(out=sr[:, :], in_=qk[s][:, 2048:3072], func=AF.Silu),
                     reads=[("qk", s)], writes=["sr"])
                S.op("pool", lambda e: e.tensor_tensor(out=sr[:, :], in0=sr[:, :], in1=gnb[:, :], op=ALU.mult),
                     reads=["sr", "gnb"], writes=["sr"])
                ys = n % 2
                for h in range(4):
                    S.op("dve", (lambda h=h: lambda e: e.scalar_tensor_tensor(
                        out=ygl[ys][:, h * 256:(h + 1) * 256], in0=psM[4 + h // 2][:, (h % 2) * 256:(h % 2 + 1) * 256],
                        scalar=rstd4[:, h:h + 1], in1=sr[:, h * 256:(h + 1) * 256], op0=ALU.mult, op1=ALU.mult))(),
                        reads=[RM[4 + h // 2], "rstd4", "sr"], writes=[("ygl", ys, h // 2)])
                S.dma("sp", lambda e: e.dma_start(out=Y_d[n * P:(n + 1) * P, 0:1024], in_=ygl[ys][:, :]),
                      "yg%d" % ys, reads=[("ygl", ys, 0), ("ygl", ys, 1)], writes=[("Y_d", n, 0)])

            cnt = [0]

            def nxt():
                cnt[0] += 1
                return cnt[0] % 2

            seq = [("c", 0, 0), ("c", 0, 1), ("c", 1, 1), ("c", 1, 0)]
            seq += [("a", 1, n + 2) for n in range(NT - 1, -1, -1)]
            seq += [("b", 0, n + 2) for n in range(NT)]
            load_tile(seq[0][2], 0)
            for i, (kind, di, tt) in enumerate(seq):
                s = i % 2
                if i + 1 < len(seq):
                    load_tile(seq[i + 1][2], (i + 1) % 2)
                if kind == "c":
                    state_step(di, s)
                elif kind == "a":
                    state_step(1, s, store=tt - 2)
                else:
                    out_step(s, tt - 2)
                    state_step(0, s)
            with nc.Block() as block:
                S.replay(block, final=(stop <= 2))
        if stop <= 2:
            return nc
        S.fence()

        es3 = ExitStack()
        with es3:
            def sb3(name, shape, dt):
                return es3.enter_context(nc.sbuf_tensor("e3_" + name, list(shape), dt))
            pall = sb3("pall", [P, NT, 1024], BF16)
            wp = sb3("wp", [P, 8, 256], BF16)
            psb = sb3("psb", [P, 1024], F32)
            dd = sb3("dd", [P, 1024], BF16)
            ddT = sb3("ddT", [P, 8, 128], BF16)
            ypl = [sb3("ypl%d" % i, [P, 1024], BF16) for i in range(2)]
            for n in range(NT):
                S.dma("sp", (lambda n=n: lambda e: e.dma_start(
                    out=pall[:, n, :], in_=U_d[(n + 2) * P:(n + 3) * P, 3072:4096]))(),
                    "pl%d" % (n % 4), reads=[("U_d", n + 2)], writes=[("pall", n)])
            S.dma("pool", lambda e: e.dma_start(
                out=wp[:, :, :].rearrange("p (g k) d -> p g k d", k=2),
                in_=w_pool.rearrange("g (k p) d -> p g k d", p=P)), "c3", writes=["wp"])
            bcast_load(psb[:, :], pool_scale[0:1, :], "b0", "psb")
            for to in range(NT):
                for g, w in enumerate(WINS):
                    lst = [(to + d, POOL_IDX[(w, d)]) for d in _pool_deltas(w) if 0 <= to + d < NT]
                    pp = psM[g // 2][:, (g % 2) * 256:(g % 2 + 1) * 256]
                    for li, (ti, mi) in enumerate(lst):
                        S.op("pe", (lambda ti=ti, mi=mi, pp=pp, li=li, L=len(lst), g=g: lambda e: e.matmul(
                            pp, lhsT=poolm(mi), rhs=pall[:, ti, g * 256:(g + 1) * 256], start=(li == 0), stop=(li == L - 1)))(),
                            reads=["cb", ("pall", ti)], writes=[RM[g // 2]], inc=(li == len(lst) - 1))
                    ic0 = CF["invcnt"][0] + to * 4 + g
                    S.op("dve", (lambda pp=pp, ic0=ic0, g=g, to=to: lambda e: e.scalar_tensor_tensor(
                        out=dd[:, g * 256:(g + 1) * 256], in0=pp, scalar=cf[:, ic0:ic0 + 1],
                        in1=pall[:, to, g * 256:(g + 1) * 256], op0=ALU.mult, op1=ALU.subtract))(),
                        reads=[RM[g // 2], "cf", ("pall", to)], writes=[("dd", g)])
                transposes(lambda i: dd[:, i * 128:(i + 1) * 128], 8,
                           lambda g0, n: ddT[:, g0:g0 + n, :], [("dd", g) for g in range(4)],
                           lambda g0: ("ddT", g0))
                for g in range(4):
                    pp = psM[2 + g // 2][:, (g % 2) * 256:(g % 2 + 1) * 256]
                    for kk in range(2):
                        S.op("pe", (lambda g=g, kk=kk, pp=pp: lambda e: e.matmul(
                            pp, lhsT=ddT[:, 2 * g + kk, :], rhs=wp[:, 2 * g + kk, :], start=(kk == 0), stop=(kk == 1)))(),
                            reads=[("ddT", 0), ("ddT", 4), "wp"], writes=[RM[2 + g // 2]], inc=(kk == 1 and g % 2 == 1))
                ys = to % 2
                for half in range(2):
                    S.op("dve", (lambda half=half, ys=ys: lambda e: e.tensor_tensor(
                        out=ypl[ys][:, half * 512:(half + 1) * 512], in0=psM[2 + half][:, :],
                        in1=psb[:, half * 512:(half + 1) * 512], op=ALU.mult))(),
                        reads=[RM[2 + half], "psb"], writes=[("ypl", ys, half)])
                S.dma("sp", (lambda to=to, ys=ys: lambda e: e.dma_start(
                    out=Y_d[to * P:(to + 1) * P, 1024:2048], in_=ypl[ys][:, :]))(),
                    "yp%d" % ys, reads=[("ypl", ys, 0), ("ypl", ys, 1)], writes=[("Y_d", to, 1)])
            with nc.Block() as block:
                S.replay(block, final=(stop <= 3))
        if stop <= 3:
            return nc
        S.fence()

        es4 = ExitStack()
        with es4:
            def sb4(name, shape, dt):
                return es4.enter_context(nc.sbuf_tensor("e4_" + name, list(shape), dt))
            wo = sb4("wo", [P, KD, D], BF16)
            wr = sb4("wr", [P, KD, NE], BF16)
            xt = [sb4("xt%d" % i, [P, D], F32) for i in range(2)]
            tmp = sb4("tmp", [P, D], F32)
            tmpB = sb4("tmpB", [P, D], F32)
            junkB = sb4("junkB", [P, D], BF16)
            ssqB = sb4("ssqB", [P, 1], F32)
            rstdB = sb4("rstdB", [P, 1], F32)
            TMPB = [("tmpB", c) for c in range(4)]
            gt1g = sb4("gt1g", [P, D], F32)
            gm2 = sb4("gm2", [P, D], F32)
            sh2 = sb4("sh2", [P, D], F32)
            yb = [sb4("yb%d" % i, [P, D], BF16) for i in range(2)]
            yTt = sb4("yTt", [P, KD, P], BF16)
            h2b = [sb4("h2b%d" % i, [P, D], BF16) for i in range(2)]
            h2Tt = sb4("h2Tt", [P, KD, P], BF16)
            junk = sb4("junk", [P, D], BF16)
            ssq = sb4("ssq", [P, 1], F32)
            rstd = sb4("rstd", [P, 1], F32)
            rbb = sb4("rbb", [P, NE], F32)
            sc = sb4("sc", [P, NE], F32)
            sel = sb4("sel", [P, NE], F32)
            m8g = sb4("m8g", [P, 64], F32)
            gs_ = sb4("gs_", [P, 8], F32)
            gm8 = sb4("gm8", [P, 8], F32)
            gmask = sb4("gmask", [P, 8], F32)
            tsel = sb4("tsel", [P, NE], F32)
            m8 = sb4("m8", [P, 8], F32)
            selm = sb4("selm", [P, NE], F32)
            selmb = sb4("selmb", [P, NE], BF16)
            Rb = sb4("Rb", [P, NE], BF16)
            Gm = sb4("Gm", [P, NE], F32)
            den = sb4("den", [P, 1], F32)
            Gt = sb4("Gt", [P, NE], F32)
            selm2 = sb4("selm2", [P, NE], F32)
            selmA = sb4("selmA", [P, NT * NE], F32)
            GtA = sb4("GtA", [P, NT * NE], F32)
            Rtot = sb4("Rtot", [P, NE], F32)

            key = sb4("key", [P, NE], F32)
            k8 = sb4("k8", [P, 8], F32)
            t8 = sb4("t8", [P, 8], F32)
            t4 = sb4("t4", [P, 4], F32)
            j64 = sb4("j64", [P, NE], F32)
            zrow = sb4("zrow", [P, D], BF16)

            for q4 in range(4):
                S.dma("pool", (lambda q4=q4: lambda e: e.dma_start(
                    out=wo[:, q4 * 4:(q4 + 1) * 4, :],
                    in_=w_out[q4 * 512:(q4 + 1) * 512, :].rearrange("(k p) n -> p k n", p=P)))(),
                    "wo%d" % (q4 % 2), writes=[("wo", q4)])
            S.dma("pool", lambda e: e.dma_start(out=wr[:, :, :], in_=w_router.rearrange("(k p) n -> p k n", p=P)),
                  "c3", writes=["wr"])
            bcast_load(rbb[:, :], router_bias[0:1, :], "b0", "rbb")
            S.op("pool", lambda e: e.memset(Rb[:, :], 0.0), writes=["Rb"])
            S.op("pool", lambda e: e.memset(Rtot[:, :], 0.0), writes=["Rtot"])
            S.op("pool", lambda e: e.memset(zrow[:, :], 0.0), writes=["zrow"])
            S.dma("sp", lambda e: e.dma_start(out=YG_d[DUMP:DUMP + 128, :], in_=zrow[:, :]), "c2",
                  reads=["zrow"], writes=["YGdump"])
            TMPALL = [("tmpc", c) for c in range(4)]
            bcast_load(tmp[:, :], nrm[1:2, :], "b1", TMPALL)
            S.dma("sp", lambda e: e.dma_start(out=gt1g[:, :], in_=modD[0:1, 2 * D:3 * D].partition_broadcast(P)), "b2",
                  writes=["gt1g"])
            S.op("dve", lambda e: e.tensor_tensor(out=gt1g[:, :], in0=gt1g[:, :], in1=tmp[:, :], op=ALU.mult),
                 reads=["gt1g"] + TMPALL, writes=["gt1g"])
            bcast_load(tmp[:, :], nrm[2:3, :], "b1", TMPALL)
            S.dma("sp", lambda e: e.dma_start(out=gm2[:, :], in_=modD[0:1, 4 * D:5 * D].partition_broadcast(P)), "b3",
                  writes=["gm2"])
            S.op("dve", lambda e: e.scalar_tensor_tensor(out=gm2[:, :], in0=gm2[:, :], scalar=1.0, in1=tmp[:, :],
                                                         op0=ALU.add, op1=ALU.mult),
                 reads=["gm2"] + TMPALL, writes=["gm2"])
            S.dma("sp", lambda e: e.dma_start(out=sh2[:, :], in_=modD[0:1, 3 * D:4 * D].partition_broadcast(P)), "b4",
                  writes=["sh2"])

            def A_ld(n):
                s = n % 2
                S.dma("sp", (lambda n=n, s=s: lambda e: e.dma_start(out=yb[s][:, :], in_=Y_d[n * P:(n + 1) * P, :]))(),
                      "yb%d" % s, reads=[("Y_d", n, 0), ("Y_d", n, 1)], writes=[("yb", s)])
                S.dma("sp", (lambda n=n, s=s: lambda e: e.dma_start(out=xt[s][:, :], in_=x[n * P:(n + 1) * P, :]))(),
                      "xt%d" % s, writes=[("xt", s)])

            def A_pe(n):
                s = n % 2
                transposes(lambda i, s=s: yb[s][:, i * 128:(i + 1) * 128], KD,
                           lambda g0, nn: yTt[:, g0:g0 + nn, :], [("yb", s)], lambda g0: ("yTt", g0))
                for cg in range(4):
                    for k in range(KD):
                        S.op("pe", (lambda cg=cg, k=k: lambda e: e.matmul(
                            psM[cg][:, :], lhsT=yTt[:, k, :], rhs=wo[:, k, cg * 512:(cg + 1) * 512],
                            start=(k == 0), stop=(k == KD - 1)))(),
                            reads=[("yTt", 4 * (k // 4)), ("wo", k // 4)], writes=[RM[cg]], inc=(k == KD - 1))
            def A_ew(n):
                s = n % 2
                for cg in range(4):
                    S.op("act", (lambda cg=cg: lambda e: e.activation(
                        out=junk[:, cg * 512:(cg + 1) * 512], in_=psM[cg][:, :], func=AF.Square,
                        accum_out=t4[:, cg:cg + 1]))(),
                        reads=[RM[cg]], writes=[("junk", cg), ("t4", cg)])
                S.op("dve", lambda e: e.tensor_reduce(out=ssq[:, :], in_=t4[:, 0:4], axis=mybir.AxisListType.X, op=ALU.add),
                     reads=[("t4", c) for c in range(4)], writes=["ssq"])
                S.op("act", lambda e: e.activation(out=ssq[:, :], in_=ssq[:, :], func=AF.Sqrt, bias=epsb[:, :], scale=1.0 / D),
                     reads=["ssq", "epsb"], writes=["ssq"])
                S.op("dve", lambda e: e.reciprocal(out=rstd[:, :], in_=ssq[:, :]), reads=["ssq"], writes=["rstd"])
                for cg in range(4):
                    S.op("dve", (lambda cg=cg: lambda e: e.scalar_tensor_tensor(
                        out=tmp[:, cg * 512:(cg + 1) * 512], in0=psM[cg][:, :], scalar=rstd[:, 0:1],
                        in1=gt1g[:, cg * 512:(cg + 1) * 512], op0=ALU.mult, op1=ALU.mult))(),
                        reads=[RM[cg], "rstd", "gt1g"], writes=[("tmpc", cg)])
                S.op("pool", (lambda s=s: lambda e: e.tensor_tensor(out=xt[s][:, :], in0=xt[s][:, :], in1=tmp[:, :], op=ALU.add))(),
                     reads=TMPALL + [("xt", s)], writes=[("xt", s)])
                S.dma("sp", (lambda n=n, s=s: lambda e: e.dma_start(out=X1_d[n * P:(n + 1) * P, :], in_=xt[s][:, :]))(),
                      "x1%d" % s, reads=[("xt", s)], writes=[("X1_d", n)])
            def B_h2(n):
                s = n % 2
                S.op("act", (lambda s=s: lambda e: e.activation(out=junkB[:, :], in_=xt[s][:, :], func=AF.Square, accum_out=ssqB[:, :]))(),
                     reads=[("xt", s)], writes=[("junkB", c) for c in range(4)] + ["ssqB"])
                S.op("act", lambda e: e.activation(out=ssqB[:, :], in_=ssqB[:, :], func=AF.Sqrt, bias=epsb[:, :], scale=1.0 / D),
                     reads=["ssqB", "epsb"], writes=["ssqB"])
                S.op("dve", lambda e: e.reciprocal(out=rstdB[:, :], in_=ssqB[:, :]), reads=["ssqB"], writes=["rstdB"])
                S.op("dve", (lambda s=s: lambda e: e.scalar_tensor_tensor(
                    out=tmpB[:, :], in0=xt[s][:, :], scalar=rstdB[:, 0:1], in1=gm2[:, :], op0=ALU.mult, op1=ALU.mult))(),
                    reads=[("xt", s), "rstdB", "gm2"], writes=TMPB)
                S.op("pool", (lambda s=s: lambda e: e.tensor_tensor(out=h2b[s][:, :], in0=tmpB[:, :], in1=sh2[:, :], op=ALU.add))(),
                     reads=TMPB + ["sh2"], writes=[("h2b", s)])
                S.dma("sp", (lambda n=n, s=s: lambda e: e.dma_start(out=H_d[n * P:(n + 1) * P, :], in_=h2b[s][:, :]))(),
                      "hd%d" % s, reads=[("h2b", s)], writes=[("H_d", n)])
            def B_pe(n):
                s = n % 2
                transposes(lambda i, s=s: h2b[s][:, i * 128:(i + 1) * 128], KD,
                           lambda g0, nn: h2Tt[:, g0:g0 + nn, :], [("h2b", s)], lambda g0: ("h2Tt", g0))
                pr = psM[4]
                for k in range(KD):
                    S.op("pe", (lambda k=k: lambda e: e.matmul(
                        pr[:, 0:NE], lhsT=h2Tt[:, k, :], rhs=wr[:, k, :], start=(k == 0), stop=(k == KD - 1)))(),
                        reads=[("h2Tt", 4 * (k // 4)), "wr"], writes=[RM[4]], inc=(k == KD - 1))
            def B_rt(n):
                s = n % 2
                pr = psM[4]
                S.op("act", lambda e: e.activation(out=sc[:, :], in_=pr[:, 0:NE], func=AF.Sigmoid), reads=[RM[4]], writes=["sc"])
                S.op("dve", lambda e: e.tensor_tensor(out=sel[:, :], in0=sc[:, :], in1=rbb[:, :], op=ALU.add),
                     reads=["sc", "rbb"], writes=["sel"])
                for g in range(8):
                    S.op("dve", (lambda g=g: lambda e: e.max(out=m8g[:, g * 8:(g + 1) * 8], in_=sel[:, g * 8:(g + 1) * 8]))(),
                         reads=["sel"], writes=[("m8g", g)])
                m8g3 = m8g[:, :].rearrange("p (g k) -> p g k", k=8)
                S.op("dve", lambda e: e.tensor_tensor(out=gs_[:, :], in0=m8g3[:, :, 0], in1=m8g3[:, :, 1], op=ALU.add),
                     reads=[("m8g", g) for g in range(8)], writes=["gs"])
                S.op("dve", lambda e: e.max(out=gm8[:, :], in_=gs_[:, :]), reads=["gs"], writes=["gm8"])
                S.op("dve", lambda e: e.tensor_scalar(out=gmask[:, :], in0=gs_[:, :], scalar1=gm8[:, 3:4], scalar2=None, op0=ALU.is_ge),
                     reads=["gs", "gm8"], writes=["gmask"])
                for g in range(8):
                    S.op("dve", (lambda g=g: lambda e: e.tensor_scalar(
                        out=tsel[:, g * 8:(g + 1) * 8], in0=sel[:, g * 8:(g + 1) * 8], scalar1=1.0, scalar2=gmask[:, g:g + 1],
                        op0=ALU.add, op1=ALU.mult))(),
                        reads=["sel", "gmask"], writes=[("tsel", g)])
                S.op("dve", lambda e: e.max(out=m8[:, :], in_=tsel[:, :]), reads=[("tsel", g) for g in range(8)], writes=["m8"])
                selm_n = selmA[:, n * NE:(n + 1) * NE]
                S.op("dve", (lambda selm_n=selm_n: lambda e: e.tensor_scalar(
                    out=selm_n, in0=tsel[:, :], scalar1=m8[:, 7:8], scalar2=None, op0=ALU.is_ge))(),
                     reads=[("tsel", g) for g in range(8)] + ["m8"], writes=[("selmA", n)])
                S.op("dve", (lambda selm_n=selm_n: lambda e: e.scalar_tensor_tensor(
                    out=Gm[:, :], in0=sc[:, :], scalar=1.0, in1=selm_n, op0=ALU.mult, op1=ALU.mult, accum_out=den[:, :]))(),
                     reads=["sc", ("selmA", n)], writes=["Gm", "den"])
                S.op("dve", lambda e: e.reciprocal(out=den[:, :], in_=den[:, :]), reads=["den"], writes=["den"])
                S.op("dve", (lambda n=n: lambda e: e.tensor_scalar(
                    out=GtA[:, n * NE:(n + 1) * NE], in0=Gm[:, :], scalar1=den[:, 0:1], scalar2=2.5, op0=ALU.mult, op1=ALU.mult))(),
                     reads=["Gm", "den"], writes=[("GtA", n)])
                S.op("pool", (lambda selm_n=selm_n: lambda e: e.tensor_tensor(out=Rtot[:, :], in0=Rtot[:, :], in1=selm_n, op=ALU.add))(),
                     reads=["Rtot", ("selmA", n)], writes=["Rtot"])

            A_ld(0)
            A_ld(1)
            A_pe(0)
            A_ew(0)
            for n in range(NT):
                if n + 1 < NT:
                    A_pe(n + 1)
                B_h2(n)
                if n + 2 < NT:
                    A_ld(n + 2)
                if n + 1 < NT:
                    A_ew(n + 1)
                B_pe(n)
                B_rt(n)

            RT_d = scratch("RT_d", [P, 2 * NT * NE + NE], F32)
            nc_rt[0] = RT_d
            S.dma("sp", lambda e: e.dma_start(out=RT_d[:, 0:NT * NE], in_=selmA[:, :]), "c2",
                  reads=[("selmA", n) for n in range(NT)], writes=["RT0"])
            S.dma("sp", lambda e: e.dma_start(out=RT_d[:, NT * NE:2 * NT * NE], in_=GtA[:, :]), "c2",
                  reads=[("GtA", n) for n in range(NT)], writes=["RT1"])
            S.dma("sp", lambda e: e.dma_start(out=RT_d[:, 2 * NT * NE:], in_=Rtot[:, :]), "c2",
                  reads=["Rtot"], writes=["RT2"])
            with nc.Block() as block:
                S.replay(block, final=(stop <= 4))
        if stop <= 4:
            return nc
        S.fence()

        es5 = ExitStack()
        with es5:
            def sb5(name, shape, dt):
                return es5.enter_context(nc.sbuf_tensor("e5_" + name, list(shape), dt))
            wg = [sb5("wg%d" % i, [P, KD, 512], BF16) for i in range(2)]
            wu = [sb5("wu%d" % i, [P, KD, 512], BF16) for i in range(2)]
            wd = [sb5("wd%d" % i, [P, 4, D], BF16) for i in range(2)]
            xg = sb5("xg", [P, NTE, D], BF16)
            xgT = sb5("xgT", [P, KD, CAP], BF16)
            aT = sb5("aTe", [P, 4, CAP], BF16)
            sg = sb5("sg", [P, 512], F32)
            ye = [sb5("ye%d" % i, [P, D], BF16) for i in range(2)]
            selmA = sb5("selmA", [P, NT * NE], F32)
            selmbA = sb5("selmbA", [P, NT * NE], BF16)
            GtA = sb5("GtA", [P, NT * NE], F32)
            Rtot = sb5("Rtot", [P, NE], F32)
            Rb = sb5("Rb", [P, NE], BF16)
            Rtotb = sb5("Rtotb", [P, NE], BF16)
            cpc = sb5("cpc", [P, 1], F32)
            cpr = sb5("cpr", [P, NE], F32)
            GTb = sb5("GTb", [P, NE], BF16)
            rkc = sb5("rkc", [P, 1], F32)
            OHb = sb5("OHb", [P, NE], BF16)
            OHTb = sb5("OHTb", [P, NE], BF16)
            ebd = sb5("ebd", [P, NE], F32)
            caprow = sb5("caprow", [P, NE], F32)
            widf = sb5("widf", [P, NE * 4], F32)
            hsc = [sb5("hsc0", [P, D], BF16)]
            selm2 = sb5("selm2", [P, NE], F32)
            key = sb5("key", [P, NE], F32)
            k8 = sb5("k8", [P, 8], F32)
            t8 = sb5("t8", [P, 8], F32)
            j64 = sb5("j64", [P, NE], F32)
            RT_d = nc_rt[0]
            S.dma("sp", lambda e: e.dma_start(out=selmA[:, :], in_=RT_d[:, 0:NT * NE]), "c2", writes=[("selmA", n) for n in range(NT)])
            S.dma("sp", lambda e: e.dma_start(out=GtA[:, :], in_=RT_d[:, NT * NE:2 * NT * NE]), "c2", writes=[("GtA", n) for n in range(NT)])
            S.dma("sp", lambda e: e.dma_start(out=Rtot[:, :], in_=RT_d[:, 2 * NT * NE:]), "c2", writes=["Rtot"])
            S.op("pool", lambda e: e.memset(Rb[:, :], 0.0), writes=["Rb"])
            S.op("act", lambda e: e.copy(out=selmbA[:, :], in_=selmA[:, :]),
                 reads=[("selmA", n) for n in range(NT)], writes=[("selmbA", n) for n in range(NT)])
            ecol_b = cb[:, NCB0:NCB0 + 128]
            tsb_b = cb[:, NCB0 + 128:NCB0 + 256]
            capb_b = cb[:, NCB0 + 256:NCB0 + 384]
            S.op("dve", lambda e: e.tensor_copy(out=Rtotb[:, :], in_=Rtot[:, :]), reads=["Rtot"], writes=["Rtotb"])
            S.op("pe", lambda e: e.matmul(psM[4][0:NE, 0:1], lhsT=Rtotb[:, :], rhs=ones_b[:, 0:1], start=True, stop=True),
                 reads=["Rtotb", "cb"], writes=[RM[4]])
            S.op("pe", lambda e: e.matmul(psM[5][:, 0:NE], lhsT=ones_b, rhs=Rtotb[:, :], start=True, stop=True),
                 reads=["Rtotb", "cb"], writes=[RM[5]])
            S.op("dve", lambda e: e.tensor_scalar(out=cpc[0:NE, :], in0=psM[4][0:NE, 0:1], scalar1=64.0,
                                                  scalar2=CFs("tbcol")[0:NE, :], op0=ALU.mult, op1=ALU.add),
                 reads=[RM[4], "cf"], writes=["cpc"])
            S.op("dve", lambda e: e.scalar_tensor_tensor(out=cpr[0:NE, :], in0=psM[5][0:NE, 0:NE], scalar=64.0,
                                                         in1=CFs("tbrow")[0:NE, :], op0=ALU.mult, op1=ALU.add),
                 reads=[RM[5], "cf"], writes=["cpr"])
            S.op("dve", lambda e: e.tensor_scalar(out=GTb[0:NE, :], in0=cpr[0:NE, :], scalar1=cpc[0:NE, 0:1], scalar2=None, op0=ALU.is_lt),
                 reads=["cpr", "cpc"], writes=["GTb"])
            S.op("dve", lambda e: e.scalar_tensor_tensor(out=j64[0:NE, :], in0=cpr[0:NE, :], scalar=cpc[0:NE, 0:1],
                                                         in1=CFs("ones64")[0:NE, :], op0=ALU.is_gt, op1=ALU.mult,
                                                         accum_out=rkc[0:NE, :]),
                 reads=["cpr", "cpc", "cf"], writes=["j64", "rkc"])
            S.op("pe", lambda e: e.matmul(psM[4][:, 0:NE], lhsT=ones_b[0:NE, :], rhs=GTb[0:NE, :], start=True, stop=True),
                 reads=["GTb", "cb", "cpc"], writes=[RM[4]])
            S.op("dve", lambda e: e.tensor_scalar(out=OHb[0:NE, :], in0=CFs("iotar")[0:NE, :], scalar1=rkc[0:NE, 0:1], scalar2=None,
                                                  op0=ALU.is_equal),
                 reads=["rkc", "cf"], writes=["OHb"])
            S.op("dve", lambda e: e.tensor_scalar(out=OHTb[0:NE, :], in0=psM[4][0:NE, 0:NE], scalar1=CFs("iotac")[0:NE, :], scalar2=None,
                                                  op0=ALU.is_equal),
                 reads=[RM[4], "cf"], writes=["OHTb"])
            S.op("pe", lambda e: e.matmul(psM[5][:, 0:NE], lhsT=ecol_b[0:NE, :], rhs=OHb[0:NE, :], start=True, stop=True),
                 reads=["OHb", "cb", "cpr"], writes=[RM[5]])
            S.op("pe", lambda e: e.matmul(psM[4][:, 0:NE], lhsT=tsb_b[0:NE, :], rhs=OHTb[0:NE, :], start=True, stop=True),
                 reads=["OHTb", "cb"], writes=[RM[4]])
            S.op("pe", lambda e: e.matmul(psM[3][:, 0:NE], lhsT=capb_b[0:NE, :], rhs=OHTb[0:NE, :], start=True, stop=True),
                 reads=["OHTb", "cb"], writes=[RM[3]])
            S.op("dve", lambda e: e.tensor_scalar(out=ebd[:, :], in0=psM[4][:, 0:NE], scalar1=128.0, scalar2=1.0, op0=ALU.mult, op1=ALU.add),
                 reads=[RM[4]], writes=["ebd"])
            S.op("dve", lambda e: e.tensor_scalar(out=caprow[:, :], in0=psM[3][:, 0:NE], scalar1=128.0, scalar2=None, op0=ALU.mult),
                 reads=[RM[3]], writes=["caprow"])
            widf3 = widf[:, :].rearrange("p (r k) -> p r k", k=4)
            for kq in range(4):
                S.op("dve", (lambda kq=kq: lambda e: e.tensor_scalar(
                    out=widf3[:, :, kq], in0=psM[5][:, 0:NE], scalar1=512.0, scalar2=CFs("kqp")[:, kq:kq + 1],
                    op0=ALU.mult, op1=ALU.add))(),
                    reads=[RM[5], "cf"], writes=[("widf", kq)])
            S.op("dve", lambda e: e.tensor_copy(out=widx[:, :], in_=widf[:, :]), reads=[("widf", kq) for kq in range(4)], writes=["widx"])

            def pass2_tile(n):
                s = 0
                selm_n = selmA[:, n * NE:(n + 1) * NE]
                selmb_n = selmbA[:, n * NE:(n + 1) * NE]
                Gt_n = GtA[:, n * NE:(n + 1) * NE]
                S.dma("sp", (lambda n=n, s=s: lambda e: e.dma_start(out=hsc[s][:, :], in_=H_d[n * P:(n + 1) * P, :]))(),
                      "hs0", reads=[("H_d", n)], writes=[("hsc", s)])
                pp = psM[5]
                S.op("pe", lambda e: e.matmul(pp[:, 0:NE], lhsT=ones_b, rhs=Rb[:, :], start=True, stop=False),
                     reads=["cb", "Rb"], writes=[RM[5]], inc=False)
                S.op("pe", (lambda selmb_n=selmb_n: lambda e: e.matmul(pp[:, 0:NE], lhsT=tri_b, rhs=selmb_n, start=False, stop=True))(),
                     reads=["cb", ("selmbA", n)], writes=[RM[5]])
                S.op("pool", (lambda selmb_n=selmb_n: lambda e: e.tensor_tensor(out=Rb[:, :], in0=Rb[:, :], in1=selmb_n, op=ALU.add))(),
                     reads=["Rb", ("selmbA", n)], writes=["Rb"])
                S.op("dve", lambda e: e.tensor_tensor(out=selm2[:, :], in0=pp[:, 0:NE], in1=caprow[:, :], op=ALU.is_lt),
                     reads=[RM[5], "caprow"], writes=["selm2"])
                S.op("dve", (lambda selm_n=selm_n: lambda e: e.tensor_tensor(out=selm2[:, :], in0=selm2[:, :], in1=selm_n, op=ALU.mult))(),
                     reads=["selm2", ("selmA", n)], writes=["selm2"])
                S.op("dve", lambda e: e.tensor_tensor(out=key[:, :], in0=pp[:, 0:NE], in1=ebd[:, :], op=ALU.add),
                     reads=[RM[5], "ebd"], writes=["key"])
                S.op("dve", lambda e: e.tensor_tensor(out=key[:, :], in0=key[:, :], in1=selm2[:, :], op=ALU.mult),
                     reads=["key", "selm2"], writes=["key"])
                S.op("dve", lambda e: e.max(out=k8[:, :], in_=key[:, :]), reads=["key"], writes=["k8"])
                for k in range(8):
                    S.op("dve", (lambda k=k, n=n, Gt_n=Gt_n: lambda e: e.scalar_tensor_tensor(
                        out=j64[:, :], in0=key[:, :], scalar=k8[:, k:k + 1], in1=Gt_n, op0=ALU.is_equal, op1=ALU.mult,
                        accum_out=gate8[:, n * 8 + k:n * 8 + k + 1]))(),
                        reads=["key", "k8", ("GtA", n)], writes=["j64", ("gate8", n)])
                S.op("dve", lambda e: e.tensor_scalar(out=t8[:, :], in0=k8[:, :], scalar1=0.0, scalar2=float(DUMP + 1),
                                                      op0=ALU.is_equal, op1=ALU.mult),
                     reads=["k8"], writes=["t8b"])
                S.op("dve", lambda e: e.tensor_tensor(out=t8[:, :], in0=t8[:, :], in1=k8[:, :], op=ALU.add),
                     reads=["t8b", "k8"], writes=["t8b"])
                S.op("dve", (lambda n=n: lambda e: e.tensor_scalar(
                    out=dsti[:, n * 8:(n + 1) * 8], in0=t8[:, :], scalar1=-1.0, scalar2=None, op0=ALU.add))(),
                    reads=["t8b"], writes=[("dsti", n)])
                for k in range(8):
                    S.dma("pool", (lambda k=k, n=n, s=s: lambda e: e.indirect_dma_start(
                        out=XG_d, out_offset=bass.IndirectOffsetOnAxis(ap=dsti[:, n * 8 + k:n * 8 + k + 1], axis=0),
                        in_=hsc[s][:, :], in_offset=None))(),
                        "sc%d" % (k % SCK), reads=[("dsti", n), ("hsc", s)], writes=["XG_all"])
            rorder = []
            for i in range(NE // 2):
                rorder += [i, NE - 1 - i]
            items = [("s", i) for i in range(4)] + [("e", r) for r in rorder]
            witems = [it for it, (kind, idx) in enumerate(items) if kind == "e" or idx == 0]

            def load_w(wi):
                kind, idx = items[witems[wi]]
                ws = wi % 2
                if kind == "s":
                    S.dma("pool", lambda e: e.dma_start(out=wg[ws][:, :, :], in_=w_sg.rearrange("(k p) f -> p k f", p=P)),
                          "wg0", writes=[("wg", ws, kq) for kq in range(4)])
                    S.dma("pool", lambda e: e.dma_start(out=wu[ws][:, :, :], in_=w_su.rearrange("(k p) f -> p k f", p=P)),
                          "wu0", writes=[("wu", ws, kq) for kq in range(4)])
                    S.dma("pool", lambda e: e.dma_start(out=wd[ws][:, :, :], in_=w_sd.rearrange("(k p) d -> p k d", p=P)),
                          "wd0", writes=[("wd", ws, kq) for kq in range(4)])
                else:
                    def gath(dst, src, kq):
                        return lambda e: e.indirect_dma_start(
                            out=dst, out_offset=None, in_=src,
                            in_offset=bass.IndirectOffsetOnAxis(ap=widx[:, idx * 4 + kq:idx * 4 + kq + 1], axis=0))
                    for kq in range(4):
                        S.dma("pool", gath(wg[ws][:, kq * 4:(kq + 1) * 4, :].rearrange("p a b -> p (a b)"), w_eg, kq),
                              "wg0", reads=["widx"], writes=[("wg", ws, kq)])
                        S.dma("pool", gath(wu[ws][:, kq * 4:(kq + 1) * 4, :].rearrange("p a b -> p (a b)"), w_eu, kq),
                              "wu0", reads=["widx"], writes=[("wu", ws, kq)])
                        S.dma("pool", gath(wd[ws][:, kq, :], w_ed, kq),
                              "wd0", reads=["widx"], writes=[("wd", ws, kq)])

            def load_x(it):
                kind, idx = items[it]
                if kind == "s":
                    src, nt = H_d[idx * 512:(idx + 1) * 512, :], 4
                else:
                    nt = CAPT[idx]
                    src = XG_d[TSTART[idx] * P:(TSTART[idx] + nt) * P, :]
                S.dma("sp", lambda e: e.dma_start(out=xg[:, 0:nt, :], in_=src.rearrange("(j p) d -> p j d", p=P)),
                      "xg0", reads=(["XG_all"] if kind == "e" else []), writes=["xg"])

            load_w(0)
            load_x(0)
            load_w(1)
            wi = -1
            yec = 0
            for it, (kind, idx) in enumerate(items):
                if kind == "e" or idx == 0:
                    wi += 1
                    if wi >= 1 and wi + 1 < len(witems):
                        load_w(wi + 1)
                ws = wi % 2
                if kind == "s":
                    for n2 in range(4 * idx, 4 * idx + 4):
                        pass2_tile(n2)
                nt = 4 if kind == "s" else CAPT[idx]
                nrow = nt * P
                groups = [(0, nrow)] if nrow <= 512 else [(0, nrow // 2), (nrow // 2, nrow)]
                tcount = 0
                for k in range(KD):
                    for j0 in range(0, nt, 4):
                        nj = min(4, nt - j0)
                        bank = tcount % 2
                        tcount += 1
                        base = 0
                        for jj in range(nj):
                            j = j0 + jj
                            S.op("pe", (lambda k=k, j=j, jj=jj, bank=bank, base=base: lambda e: e.transpose(
                                out=psT[bank][:, base + jj * 128:base + (jj + 1) * 128], in_=xg[:, j, k * 128:(k + 1) * 128],
                                identity=ident))(),
                                reads=["xg", "cb"], writes=[("psT", bank)], inc=(jj == nj - 1))
                        copy_op(evac_eng(), xgT[:, k, j0 * 128:(j0 + nj) * 128], psT[bank][:, base:base + nj * 128],
                                reads=[("psT", bank)], writes=[("xgT", k, j0)])
                if it + 1 < len(items):
                    load_x(it + 1)
                xr = lambda k: [("xgT", k, j0) for j0 in range(0, nt, 4)]
                for (r0, r1) in groups:
                    nr = r1 - r0
                    for fc in range(4):
                        for (wt_, pm_i, wn) in ((wg, 4, "wg"), (wu, 5, "wu")):
                            for k in range(KD):
                                S.op("pe", (lambda k=k, fc=fc, wt_=wt_, pm_i=pm_i, ws=ws, r0=r0, r1=r1, nr=nr: lambda e: e.matmul(
                                    psM[pm_i][:, 0:nr], lhsT=wt_[ws][:, k, fc * 128:(fc + 1) * 128], rhs=xgT[:, k, r0:r1],
                                    start=(k == 0), stop=(k == KD - 1)))(),
                                    reads=[(wn, ws, k // 4)] + xr(k), writes=[RM[pm_i]], inc=(k == KD - 1))
                        S.op("act", (lambda nr=nr: lambda e: e.activation(out=sg[:, 0:nr], in_=psM[4][:, 0:nr], func=AF.Silu))(),
                             reads=[RM[4]], writes=["sg"])
                        S.op("dve", (lambda fc=fc, r0=r0, r1=r1, nr=nr: lambda e: e.tensor_tensor(
                            out=aT[:, fc, r0:r1], in0=sg[:, 0:nr], in1=psM[5][:, 0:nr], op=ALU.mult))(),
                            reads=["sg", RM[5]], writes=[("aT", fc, r0)])
                ar = lambda fc: [("aT", fc, r0) for (r0, r1) in groups]
                for j in range(nt):
                    for dc in range(4):
                        for fc in range(4):
                            S.op("pe", (lambda j=j, dc=dc, fc=fc, ws=ws: lambda e: e.matmul(
                                psM[dc][:, :], lhsT=aT[:, fc, j * 128:(j + 1) * 128], rhs=wd[ws][:, fc, dc * 512:(dc + 1) * 512],
                                start=(fc == 0), stop=(fc == 3)))(),
                                reads=ar(fc) + [("wd", ws, fc)], writes=[RM[dc]], inc=(fc == 3))
                    ys = yec % 2
                    yec += 1
                    for dc in range(4):
                        copy_op("act" if dc % 2 == 0 else "dve", ye[ys][:, dc * 512:(dc + 1) * 512], psM[dc][:, :],
                                reads=[RM[dc]], writes=[("ye", ys, dc)])
                    if kind == "s":
                        dst = YS_d[idx * 512 + j * P:idx * 512 + (j + 1) * P, :]
                        wn_ = ("YS_d", idx * 4 + j)
                    else:
                        dst = YG_d[(TSTART[idx] + j) * P:(TSTART[idx] + j + 1) * P, :]
                        wn_ = ("YG_d", idx, j)
                    S.dma("sp", (lambda ys=ys, dst=dst: lambda e: e.dma_start(out=dst, in_=ye[ys][:, :]))(),
                          "ye%d" % ys, reads=[("ye", ys, dc) for dc in range(4)], writes=[wn_])
            with nc.Block() as block:
                S.replay(block, final=(stop <= 5))
        if stop <= 5:
            return nc
        S.fence()

        es6 = ExitStack()
        with es6:
            def sb6(name, shape, dt):
                return es6.enter_context(nc.sbuf_tensor("e6_" + name, list(shape), dt))
            xt = [sb6("xt%d" % i, [P, D], F32) for i in range(2)]
            gk = [sb6("gk%d" % i, [P, D], BF16) for i in range(8)]
            ysb = [sb6("ysb%d" % i, [P, D], BF16) for i in range(2)]
            dg = [sb6("dg%d" % i, [P, 8 * P], BF16) for i in range(2)]
            gt2g = sb6("gt2g", [P, D], F32)
            tmp = sb6("tmp", [P, D], F32)
            junk = sb6("junk", [P, D], BF16)
            t4 = sb6("t4", [P, 4], F32)
            ssq = sb6("ssq", [P, 1], F32)
            rstd = sb6("rstd", [P, 1], F32)
            TMP5 = [("tmp5", c) for c in range(4)]
            bcast_load(tmp[:, :], nrm[3:4, :], "b1", TMP5)
            S.dma("sp", lambda e: e.dma_start(out=gt2g[:, :], in_=modD[0:1, 5 * D:6 * D].partition_broadcast(P)), "b2",
                  writes=["gt2g"])
            S.op("dve", lambda e: e.tensor_tensor(out=gt2g[:, :], in0=gt2g[:, :], in1=tmp[:, :], op=ALU.mult),
                 reads=["gt2g"] + TMP5, writes=["gt2g"])
            for k in range(8):
                S.op("pool", (lambda k=k: lambda e: e.memset(gk[k][:, :], 0.0))(), writes=[("gk", k)])
            def c_load(n):
                s = n % 2
                S.dma("sp", lambda e: e.dma_start(out=ysb[s][:, :], in_=YS_d[n * P:(n + 1) * P, :]),
                      "ac%d" % s, writes=[("ysb", s)])
                S.dma("sp", lambda e: e.dma_start(out=xt[s][:, :], in_=X1_d[n * P:(n + 1) * P, :]),
                      "xt%d" % s, writes=[("xt", s)])

            c_load(0)
            for n in range(NT):
                s = n % 2
                if n + 1 < NT:
                    c_load(n + 1)
                for k in range(8):
                    S.dma("pool", (lambda k=k, n=n: lambda e: e.indirect_dma_start(
                        out=gk[k][:, :], out_offset=None, in_=YG_d,
                        in_offset=bass.IndirectOffsetOnAxis(ap=dsti[:, n * 8 + k:n * 8 + k + 1], axis=0)))(),
                        "gk%d" % (k % 3), writes=[("gk", k)])
                    S.op("dve", (lambda k=k, n=n, s=s: lambda e: e.tensor_scalar(
                        out=dg[s][:, k * P:(k + 1) * P], in0=ident, scalar1=gate8[:, n * 8 + k:n * 8 + k + 1], scalar2=None,
                        op0=ALU.mult))(),
                        reads=["cb"], writes=[("dg", s, k)])
                for c in range(4):
                    S.op("pe", (lambda c=c, s=s: lambda e: e.matmul(
                        psM[c][:, :], lhsT=ident, rhs=ysb[s][:, c * 512:(c + 1) * 512], start=True, stop=False))(),
                        reads=["cb", ("ysb", s)], writes=[RM[c]], inc=False)
                    for k in range(8):
                        S.op("pe", (lambda c=c, k=k, s=s: lambda e: e.matmul(
                            psM[c][:, :], lhsT=dg[s][:, k * P:(k + 1) * P], rhs=gk[k][:, c * 512:(c + 1) * 512],
                            start=False, stop=(k == 7)))(),
                            reads=[("dg", s, k), ("gk", k)], writes=[RM[c]], inc=(k == 7))
                for c in range(4):
                    S.op("act", (lambda c=c: lambda e: e.activation(
                        out=junk[:, c * 512:(c + 1) * 512], in_=psM[c][:, :], func=AF.Square, accum_out=t4[:, c:c + 1]))(),
                        reads=[RM[c]], writes=[("junk5", c), ("t45", c)])
                S.op("dve", lambda e: e.tensor_reduce(out=ssq[:, :], in_=t4[:, 0:4], axis=mybir.AxisListType.X, op=ALU.add),
                     reads=[("t45", c) for c in range(4)], writes=["ssq"])
                S.op("act", lambda e: e.activation(out=ssq[:, :], in_=ssq[:, :], func=AF.Sqrt, bias=epsb[:, :], scale=1.0 / D),
                     reads=["ssq", "epsb"], writes=["ssq"])
                S.op("dve", lambda e: e.reciprocal(out=rstd[:, :], in_=ssq[:, :]), reads=["ssq"], writes=["rstd"])
                for c in range(4):
                    S.op("dve", (lambda c=c: lambda e: e.scalar_tensor_tensor(
                        out=tmp[:, c * 512:(c + 1) * 512], in0=psM[c][:, :], scalar=rstd[:, 0:1],
                        in1=gt2g[:, c * 512:(c + 1) * 512], op0=ALU.mult, op1=ALU.mult))(),
                        reads=[RM[c], "rstd", "gt2g"], writes=[("tmp5", c)])
                S.op("dve", (lambda s=s: lambda e: e.tensor_tensor(out=xt[s][:, :], in0=xt[s][:, :], in1=tmp[:, :], op=ALU.add))(),
                     reads=TMP5 + [("xt", s)], writes=[("xt", s)])
                S.dma("sp", (lambda n=n, s=s: lambda e: e.dma_start(out=out[n * P:(n + 1) * P, :], in_=xt[s][:, :]))(),
                      "ou%d" % s, reads=[("xt", s)], writes=[("out", n)])
            with nc.Block() as block:
                S.replay(block, final=True)
    return nc


_CONSTS = None


def _relay(w):
    return np.ascontiguousarray(w.reshape(NE, 4, 4, 128, 512).transpose(0, 1, 3, 2, 4)).reshape(NE * 512, D)


def _prep_inputs(inputs):
    global _CONSTS
    if _CONSTS is None:
        _CONSTS = _make_consts()
    cf, cbm = _CONSTS
    f = lambda a: np.ascontiguousarray(np.asarray(a, dtype=np.float32))
    x = f(inputs["x"]); ctx = f(inputs["ctx"]); c = f(inputs["c"]); c_ctx = f(inputs["c_ctx"])
    wa2 = np.zeros((33, 1024), np.float32)
    wa2[0:16, 0:512] = f(inputs["w_a2_fwd"])[0]
    wa2[16:32, 512:1024] = f(inputs["w_a2_bwd"])[0]
    wa2[32, 0:512] = f(inputs["b_a_fwd"])[0]
    wa2[32, 512:1024] = f(inputs["b_a_bwd"])[0]
    nrm = np.concatenate([f(inputs[k]) for k in ("norm_mix_pre", "norm_mix_post", "norm_ffn_pre", "norm_ffn_post")], axis=0)
    shared = dict(
        w_mod=f(inputs["w_mod"])[0], b_mod=f(inputs["b_mod"]), nrm=nrm, w_in=f(inputs["w_in"])[0], wa2=wa2,
        gla_norm=f(inputs["gla_norm"]), w_pool=f(inputs["w_pool"])[0], pool_scale=f(inputs["pool_scale"]),
        w_out=f(inputs["w_out"])[0], w_router=f(inputs["w_router"])[0], router_bias=f(inputs["router_bias"]),
        w_eg=_relay(f(inputs["w_exp_gate"])[0]), w_eu=_relay(f(inputs["w_exp_up"])[0]),
        w_ed=f(inputs["w_exp_down"])[0].reshape(NE * 512, D),
        w_sg=f(inputs["w_sh_gate"])[0], w_su=f(inputs["w_sh_up"])[0], w_sd=f(inputs["w_sh_down"])[0],
        cf=cf, cb=cbm)
    maps = []
    for b in range(8):
        ccv = np.stack([c[b], c_ctx], axis=-1).reshape(16, 128, 2).transpose(1, 0, 2).reshape(128, 32)
        m = dict(shared)
        m.update(x=x[b], ctx=ctx[b], cc=np.ascontiguousarray(ccv))
        maps.append(m)
    return maps


def kernel(**inputs):
    maps = _prep_inputs(inputs)
    nc = build()
    res = run_bass_kernel_spmd(nc, maps, core_ids=list(range(8)))
    return np.stack([np.asarray(r["out"], dtype=np.float32) for r in res.results], axis=0)
```

```python
import os
from contextlib import ExitStack
import numpy as np
import ml_dtypes
import concourse.bass as bass
import concourse.mybir as mybir
from concourse.bass_utils import run_bass_kernel_spmd

F32 = mybir.dt.float32
BF16 = mybir.dt.bfloat16
U32 = mybir.dt.uint32
ALU = mybir.AluOpType
AF = mybir.ActivationFunctionType

P = 128
D = 2048
KD = 16
NT = 16
NTT = 18
NE = 64
CAPT = [7] + [6] * 2 + [5] * 4 + [4] * 18 + [3] * 20 + [2] * 19
assert len(CAPT) == NE
TSTART = [sum(CAPT[:i]) for i in range(NE)]
NTOT = sum(CAPT)
NTE = max(CAPT)
CAP = NTE * 128
DUMP = NTOT * 128
NROWS = NTOT * 128 + 128
EPS = 1e-6
QSCALE = 128 ** -0.5
WINS = (2, 4, 8, 16)
SCK = int(os.environ.get("SCK", "1"))

def _pool_deltas(w):
    lo, hi = -(w // 2), w // 2 - 1
    ds = []
    for d in range(-8, 9):
        ok = any(lo <= 2 * d + a - b <= hi for a in (0, 1) for b in (0, 1))
        if ok:
            ds.append(d)
    return ds

POOL_IDX = {}
_n = 0
for _w in WINS:
    for _d in _pool_deltas(_w):
        POOL_IDX[(_w, _d)] = _n
        _n += 1
NPOOLM = _n

CF = {}
_o = 0
for _name, _wd in (("TQf", 256), ("TQb", 256), ("SLf", 128), ("SLb", 128), ("MF", 512), ("MB", 512),
                   ("ones", 1), ("invcnt", 64), ("iotar", 64), ("tbrow", 64), ("iotac", 1), ("tbcol", 1),
                   ("kqp", 4), ("ones64", 64)):
    CF[_name] = (_o, _o + _wd)
    _o += _wd
NCF = _o
CB = {"ident": (0, 128), "tri": (128, 256), "ones": (256, 384), "pool": (384, 384 + 128 * NPOOLM)}
NCB0 = 384 + 128 * NPOOLM
NCB = NCB0 + 3 * 128


def _make_consts():
    j = np.arange(128)[:, None]
    i = np.arange(128)[None, :]
    mid = 64
    cf = np.zeros((128, NCF), np.float32)
    le = (j <= i).astype(np.float32)
    ge = (j >= i).astype(np.float32)
    cf[:, CF["TQf"][0]:CF["TQf"][0] + 128] = le
    cf[:, CF["TQf"][0] + 128:CF["TQf"][1]] = le - (j <= mid).astype(np.float32)
    cf[:, CF["TQb"][0]:CF["TQb"][0] + 128] = ge
    cf[:, CF["TQb"][0] + 128:CF["TQb"][1]] = ge - (j >= mid).astype(np.float32)
    cf[:, CF["SLf"][0]:CF["SLf"][1]] = (j > i)
    cf[:, CF["SLb"][0]:CF["SLb"][1]] = (j < i)
    cf[:, CF["MF"][0]:CF["MF"][1]] = np.tile(le, (1, 4))
    cf[:, CF["MB"][0]:CF["MB"][1]] = np.tile(ge, (1, 4))
    cf[:, CF["ones"][0]] = 1.0
    rows = 32
    def cnt(pos, w, n):
        return np.minimum(pos + w // 2, n) - np.maximum(pos - w // 2, 0)
    ic = np.zeros((128, 64), np.float32)
    t = np.arange(128)
    for to in range(16):
        r = 2 * to + t // 64
        c = t % 64
        for g, w in enumerate(WINS):
            ic[:, to * 4 + g] = 1.0 / (cnt(r, w, rows) * cnt(c, w, 64))
    cf[:, CF["invcnt"][0]:CF["invcnt"][1]] = ic
    cf[:, CF["iotar"][0]:CF["iotar"][1]] = np.arange(64)[None, :]
    cf[:, CF["tbrow"][0]:CF["tbrow"][1]] = (63 - np.arange(64))[None, :]
    cf[:, CF["iotac"][0]] = np.arange(128)
    cf[:, CF["tbcol"][0]] = 63 - np.arange(128)
    cf[:, CF["kqp"][0]:CF["kqp"][1]] = np.arange(4)[None, :] * 128 + np.arange(128)[:, None]
    cf[:, CF["ones64"][0]:CF["ones64"][1]] = 1.0
    cb = np.zeros((128, NCB), np.float32)
    cb[:, 0:128] = np.eye(128)
    cb[:, 128:256] = (j < i)
    cb[:, 256:384] = 1.0
    a_in = (np.arange(128) // 64)[:, None]
    c_in = (np.arange(128) % 64)[:, None]
    b_out = (np.arange(128) // 64)[None, :]
    c_out = (np.arange(128) % 64)[None, :]
    for (w, d), n in POOL_IDX.items():
        lo, hi = -(w // 2), w // 2 - 1
        dr = 2 * d + a_in - b_out
        dc = c_in - c_out
        m = ((dr >= lo) & (dr <= hi) & (dc >= lo) & (dc <= hi)).astype(np.float32)
        cb[:, 384 + n * 128:384 + (n + 1) * 128] = m
    pidx = np.arange(128)
    cb[:, NCB0:NCB0 + 128] = pidx[:, None]
    cb[:64, NCB0 + 128:NCB0 + 256] = np.array(TSTART, np.float32)[:, None]
    cb[:64, NCB0 + 256:NCB0 + 384] = np.array(CAPT, np.float32)[:, None]
    return cf, cb.astype(ml_dtypes.bfloat16)


class Tok:
    __slots__ = ("sem", "val", "own")

    def __init__(self, sem=None, val=None, own=None):
        self.sem, self.val, self.own = sem, val, own


class Sched:
    ENGS = ("pe", "act", "dve", "pool", "sp")
    LIMIT = 12000

    def __init__(self, nc, stack):
        self.nc, self.stack = nc, stack
        self.ops = {e: [] for e in self.ENGS}
        self.esem = {e: self._newsem("e_" + e) for e in self.ENGS}
        self.ecnt = {e: 0 for e in self.ENGS}
        self.pend = {e: None for e in self.ENGS}
        self.lastw, self.readers = {}, {}
        self.dsem, self.dcnt, self.dlast = {}, {}, {}
        self.all_toks = {}
        self.fence_deps = {e: [] for e in self.ENGS}
        self.nsem = 5

    def _newsem(self, name):
        return self.stack.enter_context(self.nc.semaphore(name))

    def _deps(self, reads, writes):
        deps = []
        for r in reads:
            t = self.lastw.get(r)
            if t is not None:
                deps.append(t)
        for w in writes:
            t = self.lastw.get(w)
            if t is not None:
                deps.append(t)
            deps.extend(self.readers.get(w, ()))
        return deps

    def _commit(self, tok, reads, writes):
        for r in reads:
            self.readers.setdefault(r, []).append(tok)
        for w in writes:
            self.lastw[w] = tok
            self.readers[w] = []

    def op(self, eng, fn, reads=(), writes=(), inc=True):
        deps = self._deps(reads, writes) + self.fence_deps[eng]
        self.fence_deps[eng] = []
        if inc:
            if self.ecnt[eng] >= self.LIMIT:
                self.esem[eng] = self._newsem("e_%s_%d" % (eng, self.nsem))
                self.nsem += 1
                self.ecnt[eng] = 0
            self.ecnt[eng] += 1
            tok = self.pend[eng]
            if tok is None:
                tok = Tok(own=eng)
            tok.sem, tok.val = self.esem[eng], self.ecnt[eng]
            self.pend[eng] = None
            self.all_toks[id(tok.sem)] = tok
        else:
            tok = self.pend[eng]
            if tok is None:
                tok = Tok(own=eng)
                self.pend[eng] = tok
        self.ops[eng].append((deps, fn, "c" if inc else "n", tok))
        self._commit(tok, reads, writes)
        return tok

    def dma(self, eng, fn, key, reads=(), writes=(), n=1):
        if key not in self.dsem:
            self.dsem[key] = self._newsem("d_" + str(key))
            self.nsem += 1
            self.dcnt[key] = 0
        deps = self._deps(reads, writes) + self.fence_deps[eng]
        self.fence_deps[eng] = []
        if key in self.dlast:
            deps.append(self.dlast[key])
        self.dcnt[key] += 16 * n
        tok = Tok(self.dsem[key], self.dcnt[key], own="dma")
        self.dlast[key] = tok
        self.all_toks[id(tok.sem)] = tok
        self.ops[eng].append((deps, fn, "d", tok))
        self._commit(tok, reads, writes)
        return tok

    def fence(self):
        toks = list(self.all_toks.values())
        for e in self.ENGS:
            self.fence_deps[e] = list(toks)
        self.lastw, self.readers = {}, {}

    def replay(self, block, final=False):
        nc = self.nc
        ops, self.ops = self.ops, {e: [] for e in self.ENGS}
        for e in self.ENGS:
            assert self.pend[e] is None, "unfinished group on " + e
        tail = list(self.all_toks.values()) if final else []

        def run(e, eng):
            known = self.known.setdefault(e, {})
            for deps, fn, kind, tok in ops[e]:
                for d in deps:
                    if d is tok:
                        continue
                    if e == "pe" and d.own == "pe":
                        continue
                    if known.get(id(d.sem), 0) >= d.val:
                        continue
                    eng.wait_ge(d.sem, d.val)
                    known[id(d.sem)] = d.val
                r = fn(eng)
                if kind == "c":
                    r.then_inc(tok.sem, 1)
                elif kind == "d":
                    if not isinstance(r, (list, tuple)):
                        r = [r]
                    for ins in r:
                        ins.then_inc(tok.sem, 16)
            if e == "sp":
                for d in tail:
                    if known.get(id(d.sem), 0) >= d.val:
                        continue
                    eng.wait_ge(d.sem, d.val)
                    known[id(d.sem)] = d.val

        @block.sync
        def _(eng):
            run("sp", eng)

        @block.scalar
        def _(eng):
            run("act", eng)

        @block.vector
        def _(eng):
            run("dve", eng)

        @block.tensor
        def _(eng):
            run("pe", eng)

        @block.gpsimd
        def _(eng):
            run("pool", eng)

    known = None


def build(stop=99, dbg=(), sub=9):
    nc = bass.Bass("TRN2", target_bir_lowering=False)

    def inp(name, shape, dt=F32):
        return nc.dram_tensor(name, list(shape), dt, kind="ExternalInput").ap()

    def scratch(name, shape, dt):
        kind = "ExternalOutput" if name in dbg else "Internal"
        return nc.dram_tensor(name, list(shape), dt, kind=kind).ap()

    x = inp("x", [2048, D])
    ctx = inp("ctx", [256, D])
    cc = inp("cc", [P, 32])
    w_mod = inp("w_mod", [D, 6 * D])
    b_mod = inp("b_mod", [1, 6 * D])
    nrm = inp("nrm", [4, D])
    w_in = inp("w_in", [D, 4128])
    wa2 = inp("wa2", [33, 1024])
    gla_norm = inp("gla_norm", [1, 1024])
    w_pool = inp("w_pool", [4, 256, 256])
    pool_scale = inp("pool_scale", [1, 1024])
    w_out = inp("w_out", [D, D])
    w_router = inp("w_router", [D, NE])
    router_bias = inp("router_bias", [1, NE])
    if stop >= 5:
        w_eg = inp("w_eg", [NE * 512, D])
        w_eu = inp("w_eu", [NE * 512, D])
        w_ed = inp("w_ed", [NE * 512, D])
    w_sg = inp("w_sg", [D, 512])
    w_su = inp("w_su", [D, 512])
    w_sd = inp("w_sd", [512, D])
    cf_in = inp("cf", [P, NCF])
    cb_in = inp("cb", [P, NCB], BF16)
    out = nc.dram_tensor("out", [2048, D], F32, kind="ExternalOutput").ap()

    modD = scratch("modD", [2, 6 * D], F32)
    U_d = scratch("U_d", [NTT * P, 4096], BF16)
    G_d = scratch("G_d", [NTT * P, 1024], F32)
    Y_d = scratch("Y_d", [2048, D], BF16)
    X1_d = scratch("X1_d", [2048, D], F32)
    H_d = scratch("H_d", [2048, D], BF16)
    XG_d = scratch("XG_d", [NROWS, D], BF16)
    YS_d = scratch("YS_d", [2048, D], BF16)
    YG_d = scratch("YG_d", [NROWS, D], BF16)

    stack = ExitStack()
    with stack:
        S = Sched(nc, stack)
        S.known = {}

        def sb(name, shape, dt):
            return stack.enter_context(nc.sbuf_tensor("sb_" + name, list(shape), dt))

        def ps(name, shape, dt):
            return stack.enter_context(nc.psum_tensor("ps_" + name, list(shape), dt))

        cf = sb("cf", [P, NCF], F32)
        cb = sb("cb", [P, NCB], BF16)
        dsti = sb("dsti", [P, NT * 8], U32)
        widx = sb("widx", [P, NE * 4], U32)
        gate8 = sb("gate8", [P, NT * 8], F32)

        def CFs(name, a=None, b=None):
            o0, o1 = CF[name]
            if a is not None:
                o0, o1 = o0 + a, o0 + b
            return cf[:, o0:o1]

        ident = cb[:, 0:128]
        tri_b = cb[:, 128:256]
        ones_b = cb[:, 256:384]

        def poolm(n):
            return cb[:, 384 + n * 128:384 + (n + 1) * 128]

        psT = [ps("psT%d" % i, [P, 1024], BF16) for i in range(2)]
        psM = [ps("psM%d" % i, [P, 512], F32) for i in range(6)]
        RT = [("psT", i) for i in range(2)]
        RM = [("psM", i) for i in range(6)]

        nc_rt = [None]
        S.dma("sp", lambda e: e.dma_start(out=cf[:, :], in_=cf_in), "c0", writes=["cf"])
        S.dma("sp", lambda e: e.dma_start(out=cb[:, :], in_=cb_in), "c1", writes=["cb"])

        rr = {"ev": 0}

        def evac_eng():
            rr["ev"] ^= 1
            return "act" if rr["ev"] else "dve"

        def copy_op(eng, out_ap, in_ap, reads, writes):
            if eng == "act":
                S.op("act", lambda e: e.copy(out=out_ap, in_=in_ap), reads, writes)
            elif eng == "dve":
                S.op("dve", lambda e: e.tensor_copy(out=out_ap, in_=in_ap), reads, writes)
            else:
                S.op("pool", lambda e: e.tensor_copy(out=out_ap, in_=in_ap), reads, writes)

        def bcast_load(dst, src_row, key, wname):
            wn = wname if isinstance(wname, list) else [wname]
            S.dma("sp", lambda e: e.dma_start(out=dst, in_=src_row.partition_broadcast(P)), key, writes=wn)

        def rms_rstd(src_ap, n, junk, ssq, rstd, reads, tag, junk_res=None):
            S.op("act", lambda e: e.activation(out=junk, in_=src_ap, func=AF.Square, accum_out=ssq),
                 reads=reads, writes=[junk_res if junk_res is not None else "junk" + tag, "ssq" + tag])
            S.op("act", lambda e: e.activation(out=ssq, in_=ssq, func=AF.Sqrt, bias=epsb[:, :], scale=1.0 / n),
                 reads=["ssq" + tag, "epsb"], writes=["ssq" + tag])
            S.op("dve", lambda e: e.reciprocal(out=rstd, in_=ssq),
                 reads=["ssq" + tag], writes=["rstd" + tag])

        epsb = sb("epsb", [P, 1], F32)
        ccs = sb("ccs", [P, 32], F32)
        ccb = sb("ccb", [P, 32], BF16)
        ccb3 = ccb[:, :].rearrange("p (k t) -> p k t", t=2)

        def mod_group(n, wbufs, bmt_, mods_, pm, rpm, tag, do_load=True, do_comp=True):
            s = n % len(wbufs)
            if do_load:
              S.dma("pool", lambda e: e.dma_start(
                out=wbufs[s][:, :, :], in_=w_mod[:, n * 512:(n + 1) * 512].rearrange("(k p) n -> p k n", p=P)),
                "wst%d" % s, writes=[("wst" + tag, s)])
            if not do_comp:
                return
            S.dma("sp", lambda e: e.dma_start(
                out=bmt_[:, :], in_=b_mod[0:1, n * 512:(n + 1) * 512].partition_broadcast(2)),
                "bm0", writes=["bmt" + tag])
            for k in range(KD):
                S.op("pe", (lambda k=k: lambda e: e.matmul(
                    pm[0:2, :], lhsT=ccb3[:, k, :], rhs=wbufs[s][:, k, :], start=(k == 0), stop=(k == KD - 1)))(),
                    reads=["ccb", ("wst" + tag, s)], writes=[rpm], inc=(k == KD - 1))
            S.op("dve", lambda e: e.tensor_tensor(out=mods_[:, :], in0=pm[0:2, :], in1=bmt_[:, :], op=ALU.add),
                 reads=[rpm, "bmt" + tag], writes=["mods" + tag])
            S.dma("sp", lambda e: e.dma_start(out=modD[:, n * 512:(n + 1) * 512], in_=mods_[:, :]),
                  "mo0", reads=["mods" + tag], writes=[("modD", n // 4)])
        S.op("dve", lambda e: e.memset(epsb[:, :], EPS), writes=["epsb"])

        def transposes(src_fn, nblk, dst_fn, reads, wname_fn, group=4):
            for g0 in range(0, nblk, group):
                n = min(group, nblk - g0)
                bank = (g0 // group) % 2
                base = 0
                for ii in range(n):
                    i = g0 + ii
                    o = psT[bank][:, base + ii * 128:base + (ii + 1) * 128]
                    S.op("pe", (lambda o=o, i=i: lambda e: e.transpose(out=o, in_=src_fn(i), identity=ident))(),
                         reads=list(reads) + ["cb"], writes=[("psT", bank)], inc=(ii == n - 1))
                src = psT[bank][:, base:base + n * 128].rearrange("p (a b) -> p a b", b=128)
                copy_op(evac_eng(), dst_fn(g0, n), src, reads=[("psT", bank)], writes=[wname_fn(g0)])

        es1 = ExitStack()
        with es1:
            def sb1(name, shape, dt):
                return es1.enter_context(nc.sbuf_tensor("e1_" + name, list(shape), dt))
            wst = [sb1("wst%d" % i, [P, KD, 512], BF16) for i in range(2)]
            bmt = [sb1("bmt0", [2, 512], F32)] * 2
            mods = [sb1("mods0", [2, 512], F32)] * 2
            gm1 = sb1("gm1", [P, D], F32)
            sh1 = sb1("sh1", [P, D], F32)
            cgm1 = sb1("cgm1", [P, D], F32)
            csh1 = sb1("csh1", [P, D], F32)
            xt = [sb1("xt%d" % i, [P, D], F32) for i in range(2)]
            hb = [sb1("hb%d" % i, [P, D], BF16) for i in range(2)]
            ssq = sb1("ssq", [P, 1], F32)
            rstd = sb1("rstd", [P, 1], F32)
            hT = sb1("hT", [P, KD, NTT * P], BF16)
            ust = [sb1("ust%d" % i, [P, 512], BF16) for i in range(2)]
            ab = sb1("ab", [P, 32], BF16)
            aT = sb1("aT", [33, P], BF16)
            wa2b = sb1("wa2b", [33, 1024], BF16)
            ez = sb1("ez", [P, 1024], F32)
            gst = [ez, ez]
            wmx = sb1("wmx", [P, KD, 512], BF16)

            S.dma("sp", lambda e: e.dma_start(out=ccs[:, :], in_=cc), "c2", writes=["ccs"])
            S.op("act", lambda e: e.activation(out=ccb[:, :], in_=ccs[:, :], func=AF.Silu), reads=["ccs"], writes=["ccb"])
            for n in range(8):
                mod_group(n, wst, bmt[0], mods[0], psM[n % 2], RM[n % 2], "1")

            if sub <= 1:
                with nc.Block() as block:
                    S.replay(block, final=True)
                return nc
            def modrow(r, c):
                return modD[r:r + 1, c * D:(c + 1) * D]
            tmpA = xt[0]
            bcast_load(tmpA[:, :], nrm[0:1, :], "b0", ("xt", 0))
            def mk_gm(dst, dname, row, key):
                S.dma("sp", lambda e: e.dma_start(out=dst[:, :], in_=modrow(row, 1).partition_broadcast(P)), key,
                      reads=[("modD", 1)], writes=[dname])
                S.op("dve", lambda e: e.scalar_tensor_tensor(out=dst[:, :], in0=dst[:, :], scalar=1.0, in1=tmpA[:, :],
                                                             op0=ALU.add, op1=ALU.mult),
                     reads=[dname, ("xt", 0)], writes=[dname])
            mk_gm(gm1, "gm1", 0, "b1")
            mk_gm(cgm1, "cgm1", 1, "b2")
            S.dma("sp", lambda e: e.dma_start(out=sh1[:, :], in_=modrow(0, 0).partition_broadcast(P)), "b3",
                  reads=[("modD", 0)], writes=["sh1"])
            S.dma("sp", lambda e: e.dma_start(out=csh1[:, :], in_=modrow(1, 0).partition_broadcast(P)), "b4",
                  reads=[("modD", 0)], writes=["csh1"])

            S.dma("pool", lambda e: e.dma_start(out=wa2b[:, :], in_=wa2), "c3", writes=["wa2b"])
            S.op("pool", lambda e: e.memset(aT[32:33, :], 1.0), writes=["aT1"])
            hT_reads = lambda tt: [("hT", tt, g0) for g0 in range(0, KD, 4)]
            groups = [(0, 512, 0), (512, 512, 512), (1024, 512, 1024), (1536, 512, 1536),
                      (2048, 512, 2048), (2560, 512, 2560), (3072, 32, None), (3104, 512, 3072), (3616, 512, 3584)]
            mmr = [0]
            def win_load(gi):
                c0, ncol, u0 = groups[gi]
                s = gi % 2
                S.dma("pool", lambda e: e.dma_start(
                    out=wst[s][:, :, 0:ncol], in_=w_in[:, c0:c0 + ncol].rearrange("(k p) n -> p k n", p=P)),
                    "wst%d" % s, writes=[("wst1", s)])

            def win_block(gi, tt):
                c0, ncol, u0 = groups[gi]
                s = gi % 2
                if True:
                    pm = psM[2 + mmr[0] % 4]
                    rpm = RM[2 + mmr[0] % 4]
                    mmr[0] += 1
                    for k in range(KD):
                        S.op("pe", (lambda k=k, s=s, pm=pm, tt=tt, ncol=ncol: lambda e: e.matmul(
                            pm[:, 0:ncol], lhsT=hT[:, k, tt * P:(tt + 1) * P], rhs=wst[s][:, k, 0:ncol],
                            start=(k == 0), stop=(k == KD - 1)))(),
                            reads=hT_reads(tt) + [("wst1", s)], writes=[rpm], inc=(k == KD - 1))
                    if u0 is not None:
                        us = mmr[0] % 2
                        copy_op(evac_eng(), ust[us][:, :], pm[:, :], reads=[rpm], writes=[("ust", us)])
                        S.dma("sp", (lambda us=us, tt=tt, u0=u0: lambda e: e.dma_start(
                            out=U_d[tt * P:(tt + 1) * P, u0:u0 + 512], in_=ust[us][:, :]))(),
                            "us%d" % us, reads=[("ust", us)], writes=[("U_d", tt)])
                    else:
                        gs = tt % 2
                        S.op("act", (lambda pm=pm: lambda e: e.copy(out=ab[:, :], in_=pm[:, 0:32]))(),
                             reads=[rpm], writes=["ab"])
                        S.op("pe", lambda e: e.transpose(out=psT[0][0:32, 0:128], in_=ab[:, :], identity=ident),
                             reads=["ab", "cb"], writes=[("psT", 0)])
                        S.op("dve", lambda e: e.tensor_copy(out=aT[0:32, :], in_=psT[0][0:32, 0:128]),
                             reads=[("psT", 0)], writes=["aT"])
                        for hh in range(2):
                            S.op("pe", (lambda hh=hh: lambda e: e.matmul(
                                psM[hh][:, :], lhsT=aT[:, :], rhs=wa2b[:, hh * 512:(hh + 1) * 512], start=True, stop=True))(),
                                reads=["aT", "aT1", "wa2b"], writes=[RM[hh]])
                            S.op("act", (lambda hh=hh: lambda e: e.activation(
                                out=ez[:, hh * 512:(hh + 1) * 512], in_=psM[hh][:, :], func=AF.Exp, scale=-1.0))(),
                                reads=[RM[hh]], writes=[("ez", hh)])
                            S.op("act", (lambda hh=hh: lambda e: e.activation(
                                out=ez[:, hh * 512:(hh + 1) * 512], in_=ez[:, hh * 512:(hh + 1) * 512], func=AF.Ln,
                                bias=1.0, scale=1.0))(),
                                reads=[("ez", hh)], writes=[("ez", hh)])
                        S.op("dve", (lambda gs=gs: lambda e: e.tensor_scalar(
                            out=gst[gs][:, :], in0=ez[:, :], scalar1=-1.0 / 16.0, scalar2=None, op0=ALU.mult))(),
                            reads=[("ez", 0), ("ez", 1)], writes=[("ez", 0), ("ez", 1)])
                        S.dma("sp", (lambda gs=gs, tt=tt: lambda e: e.dma_start(
                            out=G_d[tt * P:(tt + 1) * P, :], in_=gst[gs][:, :]))(),
                            "gs0", reads=[("ez", 0), ("ez", 1)], writes=[("G_d", tt)])
            win_load(0)
            win_load(1)
            def h_load(tt):
                s = tt % 2
                src = ctx[tt * P:(tt + 1) * P, :] if tt < 2 else x[(tt - 2) * P:(tt - 1) * P, :]
                S.dma("sp", lambda e: e.dma_start(out=xt[s][:, :], in_=src), "xt%d" % s, writes=[("xt", s)])

            h_load(0)
            for tt in range(NTT):
                s = tt % 2
                if tt + 1 < NTT:
                    h_load(tt + 1)
                if tt >= 1 and sub > 2:
                    win_block(0, tt - 1)
                rms_rstd(xt[s][:, :], D, hb[s][:, :], ssq[:, :], rstd[:, :], [("xt", s)], "1", junk_res=("hb", s))
                g_, s_, gn, sn = (cgm1, csh1, "cgm1", "csh1") if tt < 2 else (gm1, sh1, "gm1", "sh1")
                S.op("dve", (lambda s=s, g_=g_: lambda e: e.scalar_tensor_tensor(
                    out=xt[s][:, :], in0=xt[s][:, :], scalar=rstd[:, 0:1], in1=g_[:, :], op0=ALU.mult, op1=ALU.mult))(),
                    reads=[("xt", s), "rstd1", gn], writes=[("xt", s)])
                S.op("dve", (lambda s=s, s_=s_: lambda e: e.tensor_tensor(
                    out=hb[s][:, :], in0=xt[s][:, :], in1=s_[:, :], op=ALU.add))(),
                    reads=[("xt", s), sn], writes=[("hb", s)])
                transposes(lambda i, s=s: hb[s][:, i * 128:(i + 1) * 128], KD,
                           lambda g0, n, tt=tt: hT[:, g0:g0 + n, tt * P:(tt + 1) * P],
                           [("hb", s)], lambda g0, tt=tt: ("hT", tt, g0))

            if sub <= 2:
                with nc.Block() as block:
                    S.replay(block, final=True)
                return nc
            win_block(0, NTT - 1)
            nb = 0
            mg = 8
            for gi in range(1, len(groups)):
                if gi + 1 < len(groups):
                    win_load(gi + 1)
                for tt in range(NTT):
                    win_block(gi, tt)
                    nb += 1
                    if nb % 8 == 0 and mg < 24:
                        mod_group(mg, [wmx], bmt[0], mods[0], psM[mg % 2], RM[mg % 2], "x")
                        mg += 1
            while mg < 24:
                mod_group(mg, [wmx], bmt[0], mods[0], psM[mg % 2], RM[mg % 2], "x")
                mg += 1
            with nc.Block() as block:
                S.replay(block, final=(stop <= 1))
        if stop <= 1:
            return nc
        S.fence()

        es2 = ExitStack()
        with es2:
            def sb2(name, shape, dt):
                return es2.enter_context(nc.sbuf_tensor("e2_" + name, list(shape), dt))
            S32 = sb2("S32", [P, 8, 256], F32)
            Sbf = sb2("Sbf", [P, 8, 256], BF16)
            SbSt = sb2("SbSt", [P, NT, 1024], BF16)
            qk = [sb2("qk%d" % i, [P, 3072], BF16) for i in range(2)]
            gg = [sb2("gg%d" % i, [P, 1024], F32) for i in range(2)]
            ehat = sb2("ehat", [P, 512], F32)
            khat = sb2("khat", [P, 512], BF16)
            dec = sb2("dec", [P, 4], F32)
            eG = [sb2("eG%d" % i, [P, 512], F32) for i in range(2)]
            eq = [sb2("eq%d" % i, [P, 512], F32) for i in range(2)]
            ek = [sb2("ek%d" % i, [P, 512], F32) for i in range(2)]
            qh = [sb2("qh%d" % i, [P, 512], BF16) for i in range(2)]
            qt = [sb2("qt%d" % i, [P, 512], BF16) for i in range(2)]
            kt = [sb2("kt%d" % i, [P, 512], BF16) for i in range(2)]
            t1 = sb2("t1", [P, 512], F32)
            t2 = sb2("t2", [P, 512], F32)
            ATb = sb2("ATb", [P, 512], BF16)
            sr = sb2("sr", [P, 1024], F32)
            gnb = sb2("gnb", [P, 1024], F32)
            ygl = [sb2("ygl%d" % i, [P, 1024], BF16) for i in range(2)]
            junk2 = sb2("junk2", [P, 1024], BF16)
            dq = sb2("dq", [P, 4], F32)
            rstd4 = sb2("rstd4", [P, 4], F32)

            S.op("dve", lambda e: e.memset(S32[:, :, :], 0.0), writes=["S32f", "S32b"])
            S.op("pool", lambda e: e.memset(Sbf[:, :, :], 0.0), writes=["Sbff", "Sbfb"])
            bcast_load(gnb[:, :], gla_norm[0:1, :], "b0", "gnb")

            def load_tile(tt, s):
                S.dma("sp", lambda e: e.dma_start(out=qk[s][:, :], in_=U_d[tt * P:(tt + 1) * P, 0:3072]),
                      "qk%d" % s, reads=[("U_d", tt)], writes=[("qk", s)])
                S.dma("sp", lambda e: e.dma_start(out=gg[s][:, :], in_=G_d[tt * P:(tt + 1) * P, :]),
                      "gg%d" % s, reads=[("G_d", tt)], writes=[("gg", s)])

            def state_step(di, s, store=None):
                dn = "fb"[di]
                SL = CFs("SLf") if di == 0 else CFs("SLb")
                g_dir = gg[s][:, di * 512:(di + 1) * 512]
                S.op("pe", lambda e: e.matmul(psM[5][:, :], lhsT=SL, rhs=g_dir, start=True, stop=True),
                     reads=["cf", ("gg", s)], writes=[RM[5]])
                S.op("act", lambda e: e.activation(out=ehat[:, :], in_=psM[5][:, :], func=AF.Exp),
                     reads=[RM[5]], writes=["ehat"])
                S.op("dve", lambda e: e.tensor_tensor(out=khat[:, :], in0=qk[s][:, 512:1024], in1=ehat[:, :], op=ALU.mult),
                     reads=["ehat", ("qk", s)], writes=["khat"])
                for h in range(4):
                    S.op("pe", (lambda h=h: lambda e: e.matmul(
                        psM[5][:, h:h + 1], lhsT=gg[s][:, di * 512 + h * 128:di * 512 + (h + 1) * 128],
                        rhs=CFs("ones"), start=True, stop=True))(),
                        reads=["cf", ("gg", s)], writes=[RM[5]], inc=(h == 3))
                S.op("act", lambda e: e.activation(out=dec[:, :], in_=psM[5][:, 0:4], func=AF.Exp),
                     reads=[RM[5]], writes=["dec"])
                for h in range(4):
                    S.op("pe", (lambda h=h: lambda e: e.matmul(
                        psM[3 + h // 2][:, (h % 2) * 256:(h % 2 + 1) * 256], lhsT=khat[:, h * 128:(h + 1) * 128],
                        rhs=qk[s][:, 1024 + h * 256:1024 + (h + 1) * 256], start=True, stop=True))(),
                        reads=["khat", ("qk", s)], writes=[RM[3 + h // 2]], inc=(h % 2 == 1))
                if store is not None:
                    S.op("pool", lambda e: e.tensor_copy(out=SbSt[:, store, :],
                                                        in_=Sbf[:, di * 4:(di + 1) * 4, :].rearrange("p a b -> p (a b)")),
                         reads=["Sbf" + dn], writes=[("SbSt", store)])
                for h in range(4):
                    S.op("dve", (lambda h=h: lambda e: e.scalar_tensor_tensor(
                        out=S32[:, di * 4 + h, :], in0=S32[:, di * 4 + h, :], scalar=dec[:, h:h + 1],
                        in1=psM[3 + h // 2][:, (h % 2) * 256:(h % 2 + 1) * 256], op0=ALU.mult, op1=ALU.add))(),
                        reads=["S32" + dn, "dec", RM[3 + h // 2]], writes=["S32" + dn])
                S.op("act", lambda e: e.copy(out=Sbf[:, di * 4:(di + 1) * 4, :], in_=S32[:, di * 4:(di + 1) * 4, :]),
                     reads=["S32" + dn], writes=["Sbf" + dn])

            def out_step(s, n):
                for i in range(8):
                    S.op("pe", (lambda i=i: lambda e: e.transpose(
                        out=psT[0][:, i * 128:(i + 1) * 128], in_=qk[s][:, i * 128:(i + 1) * 128], identity=ident))(),
                        reads=[("qk", s), "cb"], writes=[("psT", 0)], inc=(i == 7))
                for di in range(2):
                    TQ = CFs("TQf") if di == 0 else CFs("TQb")
                    for h in range(4):
                        S.op("pe", (lambda h=h, di=di, TQ=TQ: lambda e: e.matmul(
                            psM[h // 2][:, (h % 2) * 256:(h % 2 + 1) * 256],
                            lhsT=gg[s][:, di * 512 + h * 128:di * 512 + (h + 1) * 128], rhs=TQ, start=True, stop=True))(),
                            reads=["cf", ("gg", s)], writes=[RM[h // 2]], inc=(h % 2 == 1))
                    for half in range(2):
                        pv = psM[half][:, :].rearrange("p (a b) -> p a b", b=256)
                        o3 = (lambda half: lambda t: t[:, half * 256:(half + 1) * 256].rearrange("p (a b) -> p a b", b=128))(half)
                        S.op("act", (lambda pv=pv, o3=o3, di=di: lambda e: e.activation(
                            out=o3(eG[di]), in_=pv[:, :, 0:128], func=AF.Exp))(),
                            reads=[RM[half]], writes=[("eG", di, half)])
                        S.op("act", (lambda pv=pv, o3=o3, di=di: lambda e: e.activation(
                            out=o3(eq[di]), in_=pv[:, :, 128:256], func=AF.Exp))(),
                            reads=[RM[half]], writes=[("eq", di, half)])
                        S.op("act", (lambda pv=pv, o3=o3, di=di: lambda e: e.activation(
                            out=o3(ek[di]), in_=pv[:, :, 128:256], func=AF.Exp, scale=-1.0))(),
                            reads=[RM[half]], writes=[("ek", di, half)])
                    rd = lambda nm: [(nm, di, 0), (nm, di, 1), ("psT", 0)]
                    S.op("dve", (lambda di=di: lambda e: e.scalar_tensor_tensor(
                        out=qh[di][:, :], in0=psT[0][:, 0:512], scalar=QSCALE, in1=eG[di][:, :], op0=ALU.mult, op1=ALU.mult))(),
                        reads=rd("eG"), writes=[("qh", di)])
                    S.op("dve", (lambda di=di: lambda e: e.scalar_tensor_tensor(
                        out=qt[di][:, :], in0=psT[0][:, 0:512], scalar=QSCALE, in1=eq[di][:, :], op0=ALU.mult, op1=ALU.mult))(),
                        reads=rd("eq"), writes=[("qt", di)])
                    S.op("dve", (lambda di=di: lambda e: e.tensor_tensor(
                        out=kt[di][:, :], in0=psT[0][:, 512:1024], in1=ek[di][:, :], op=ALU.mult))(),
                        reads=rd("ek"), writes=[("kt", di)])
                    for h in range(4):
                        S.op("pe", (lambda h=h, di=di: lambda e: e.matmul(
                            psM[2 + di][:, h * 128:(h + 1) * 128], lhsT=kt[di][:, h * 128:(h + 1) * 128],
                            rhs=qt[di][:, h * 128:(h + 1) * 128], start=True, stop=True))(),
                            reads=[("kt", di), ("qt", di)], writes=[RM[2 + di]], inc=(h == 3))
                S.op("dve", lambda e: e.tensor_tensor(out=t1[:, :], in0=psM[2][:, :], in1=CFs("MF"), op=ALU.mult),
                     reads=[RM[2], "cf"], writes=["t1"])
                S.op("dve", lambda e: e.tensor_tensor(out=t2[:, :], in0=psM[3][:, :], in1=CFs("MB"), op=ALU.mult),
                     reads=[RM[3], "cf"], writes=["t2"])
                S.op("pool", lambda e: e.tensor_tensor(out=ATb[:, :], in0=t1[:, :], in1=t2[:, :], op=ALU.add),
                     reads=["t1", "t2"], writes=["ATb"])
                for h in range(4):
                    po = psM[4 + h // 2][:, (h % 2) * 256:(h % 2 + 1) * 256]
                    rpo = RM[4 + h // 2]
                    S.op("pe", (lambda h=h, po=po: lambda e: e.matmul(
                        po, lhsT=qh[0][:, h * 128:(h + 1) * 128], rhs=Sbf[:, h, :], start=True, stop=False))(),
                        reads=[("qh", 0), "Sbff"], writes=[rpo], inc=False)
                    S.op("pe", (lambda h=h, po=po: lambda e: e.matmul(
                        po, lhsT=qh[1][:, h * 128:(h + 1) * 128], rhs=SbSt[:, n, h * 256:(h + 1) * 256], start=False, stop=False))(),
                        reads=[("qh", 1), ("SbSt", n)], writes=[rpo], inc=False)
                    S.op("pe", (lambda h=h, po=po: lambda e: e.matmul(
                        po, lhsT=ATb[:, h * 128:(h + 1) * 128], rhs=qk[s][:, 1024 + h * 256:1024 + (h + 1) * 256],
                        start=False, stop=True))(),
                        reads=["ATb", ("qk", s)], writes=[rpo], inc=(h % 2 == 1))
                for h in range(4):
                    S.op("act", (lambda h=h: lambda e: e.activation(
                        out=junk2[:, h * 256:(h + 1) * 256], in_=psM[4 + h // 2][:, (h % 2) * 256:(h % 2 + 1) * 256],
                        func=AF.Square, accum_out=dq[:, h:h + 1]))(),
                        reads=[RM[4 + h // 2]], writes=[("junk2", h), ("dq", h)])
                S.op("act", lambda e: e.activation(out=dq[:, :], in_=dq[:, :], func=AF.Sqrt, bias=epsb[:, :], scale=1.0 / 256),
                     reads=[("dq", h) for h in range(4)] + ["epsb"], writes=[("dq", h) for h in range(4)])
                S.op("dve", lambda e: e.reciprocal(out=rstd4[:, :], in_=dq[:, :]),
                     reads=[("dq", h) for h in range(4)], writes=["rstd4"])
                S.op("act", lambda e: e.activation(out=sr[:, :], in_=qk[s][:, 2048:3072], func=AF.Silu),
                     reads=[("qk", s)], writes=["sr"])
                S.op("pool", lambda e: e.tensor_tensor(out=sr[:, :], in0=sr[:, :], in1=gnb[:, :], op=ALU.mult),
                     reads=["sr", "gnb"], writes=["sr"])
                ys = n % 2
                for h in range(4):
                    S.op("dve", (lambda h=h: lambda e: e.scalar_tensor_tensor(
                        out=ygl[ys][:, h * 256:(h + 1) * 256], in0=psM[4 + h // 2][:, (h % 2) * 256:(h % 2 + 1) * 256],
                        scalar=rstd4[:, h:h + 1], in1=sr[:, h * 256:(h + 1) * 256], op0=ALU.mult, op1=ALU.mult))(),
                        reads=[RM[4 + h // 2], "rstd4", "sr"], writes=[("ygl", ys, h // 2)])
                S.dma("sp", lambda e: e.dma_start(out=Y_d[n * P:(n + 1) * P, 0:1024], in_=ygl[ys][:, :]),
                      "yg%d" % ys, reads=[("ygl", ys, 0), ("ygl", ys, 1)], writes=[("Y_d", n, 0)])

            cnt = [0]

            def nxt():
                cnt[0] += 1
                return cnt[0] % 2

            seq = [("c", 0, 0), ("c", 0, 1), ("c", 1, 1), ("c", 1, 0)]
            seq += [("a", 1, n + 2) for n in range(NT - 1, -1, -1)]
            seq += [("b", 0, n + 2) for n in range(NT)]
            load_tile(seq[0][2], 0)
            for i, (kind, di, tt) in enumerate(seq):
                s = i % 2
                if i + 1 < len(seq):
                    load_tile(seq[i + 1][2], (i + 1) % 2)
                if kind == "c":
                    state_step(di, s)
                elif kind == "a":
                    state_step(1, s, store=tt - 2)
                else:
                    out_step(s, tt - 2)
                    state_step(0, s)
            with nc.Block() as block:
                S.replay(block, final=(stop <= 2))
        if stop <= 2:
            return nc
        S.fence()

        es3 = ExitStack()
        with es3:
            def sb3(name, shape, dt):
                return es3.enter_context(nc.sbuf_tensor("e3_" + name, list(shape), dt))
            pall = sb3("pall", [P, NT, 1024], BF16)
            wp = sb3("wp", [P, 8, 256], BF16)
            psb = sb3("psb", [P, 1024], F32)
            dd = sb3("dd", [P, 1024], BF16)
            ddT = sb3("ddT", [P, 8, 128], BF16)
            ypl = [sb3("ypl%d" % i, [P, 1024], BF16) for i in range(2)]
            for n in range(NT):
                S.dma("sp", (lambda n=n: lambda e: e.dma_start(
                    out=pall[:, n, :], in_=U_d[(n + 2) * P:(n + 3) * P, 3072:4096]))(),
                    "pl%d" % (n % 4), reads=[("U_d", n + 2)], writes=[("pall", n)])
            S.dma("pool", lambda e: e.dma_start(
                out=wp[:, :, :].rearrange("p (g k) d -> p g k d", k=2),
                in_=w_pool.rearrange("g (k p) d -> p g k d", p=P)), "c3", writes=["wp"])
            bcast_load(psb[:, :], pool_scale[0:1, :], "b0", "psb")
            for to in range(NT):
                for g, w in enumerate(WINS):
                    lst = [(to + d, POOL_IDX[(w, d)]) for d in _pool_deltas(w) if 0 <= to + d < NT]
                    pp = psM[g // 2][:, (g % 2) * 256:(g % 2 + 1) * 256]
                    for li, (ti, mi) in enumerate(lst):
                        S.op("pe", (lambda ti=ti, mi=mi, pp=pp, li=li, L=len(lst), g=g: lambda e: e.matmul(
                            pp, lhsT=poolm(mi), rhs=pall[:, ti, g * 256:(g + 1) * 256], start=(li == 0), stop=(li == L - 1)))(),
                            reads=["cb", ("pall", ti)], writes=[RM[g // 2]], inc=(li == len(lst) - 1))
                    ic0 = CF["invcnt"][0] + to * 4 + g
                    S.op("dve", (lambda pp=pp, ic0=ic0, g=g, to=to: lambda e: e.scalar_tensor_tensor(
                        out=dd[:, g * 256:(g + 1) * 256], in0=pp, scalar=cf[:, ic0:ic0 + 1],
                        in1=pall[:, to, g * 256:(g + 1) * 256], op0=ALU.mult, op1=ALU.subtract))(),
                        reads=[RM[g // 2], "cf", ("pall", to)], writes=[("dd", g)])
                transposes(lambda i: dd[:, i * 128:(i + 1) * 128], 8,
                           lambda g0, n: ddT[:, g0:g0 + n, :], [("dd", g) for g in range(4)],
                           lambda g0: ("ddT", g0))
                for g in range(4):
                    pp = psM[2 + g // 2][:, (g % 2) * 256:(g % 2 + 1) * 256]
                    for kk in range(2):
                        S.op("pe", (lambda g=g, kk=kk, pp=pp: lambda e: e.matmul(
                            pp, lhsT=ddT[:, 2 * g + kk, :], rhs=wp[:, 2 * g + kk, :], start=(kk == 0), stop=(kk == 1)))(),
                            reads=[("ddT", 0), ("ddT", 4), "wp"], writes=[RM[2 + g // 2]], inc=(kk == 1 and g % 2 == 1))
                ys = to % 2
                for half in range(2):
                    S.op("dve", (lambda half=half, ys=ys: lambda e: e.tensor_tensor(
                        out=ypl[ys][:, half * 512:(half + 1) * 512], in0=psM[2 + half][:, :],
                        in1=psb[:, half * 512:(half + 1) * 512], op=ALU.mult))(),
                        reads=[RM[2 + half], "psb"], writes=[("ypl", ys, half)])
                S.dma("sp", (lambda to=to, ys=ys: lambda e: e.dma_start(
                    out=Y_d[to * P:(to + 1) * P, 1024:2048], in_=ypl[ys][:, :]))(),
                    "yp%d" % ys, reads=[("ypl", ys, 0), ("ypl", ys, 1)], writes=[("Y_d", to, 1)])
            with nc.Block() as block:
                S.replay(block, final=(stop <= 3))
        if stop <= 3:
            return nc
        S.fence()

        es4 = ExitStack()
        with es4:
            def sb4(name, shape, dt):
                return es4.enter_context(nc.sbuf_tensor("e4_" + name, list(shape), dt))
            wo = sb4("wo", [P, KD, D], BF16)
            wr = sb4("wr", [P, KD, NE], BF16)
            xt = [sb4("xt%d" % i, [P, D], F32) for i in range(2)]
            tmp = sb4("tmp", [P, D], F32)
            tmpB = sb4("tmpB", [P, D], F32)
            junkB = sb4("junkB", [P, D], BF16)
            ssqB = sb4("ssqB", [P, 1], F32)
            rstdB = sb4("rstdB", [P, 1], F32)
            TMPB = [("tmpB", c) for c in range(4)]
            gt1g = sb4("gt1g", [P, D], F32)
            gm2 = sb4("gm2", [P, D], F32)
            sh2 = sb4("sh2", [P, D], F32)
            yb = [sb4("yb%d" % i, [P, D], BF16) for i in range(2)]
            yTt = sb4("yTt", [P, KD, P], BF16)
            h2b = [sb4("h2b%d" % i, [P, D], BF16) for i in range(2)]
            h2Tt = sb4("h2Tt", [P, KD, P], BF16)
            junk = sb4("junk", [P, D], BF16)
            ssq = sb4("ssq", [P, 1], F32)
            rstd = sb4("rstd", [P, 1], F32)
            rbb = sb4("rbb", [P, NE], F32)
            sc = sb4("sc", [P, NE], F32)
            sel = sb4("sel", [P, NE], F32)
            m8g = sb4("m8g", [P, 64], F32)
            gs_ = sb4("gs_", [P, 8], F32)
            gm8 = sb4("gm8", [P, 8], F32)
            gmask = sb4("gmask", [P, 8], F32)
            tsel = sb4("tsel", [P, NE], F32)
            m8 = sb4("m8", [P, 8], F32)
            selm = sb4("selm", [P, NE], F32)
            selmb = sb4("selmb", [P, NE], BF16)
            Rb = sb4("Rb", [P, NE], BF16)
            Gm = sb4("Gm", [P, NE], F32)
            den = sb4("den", [P, 1], F32)
            Gt = sb4("Gt", [P, NE], F32)
            selm2 = sb4("selm2", [P, NE], F32)
            selmA = sb4("selmA", [P, NT * NE], F32)
            GtA = sb4("GtA", [P, NT * NE], F32)
            Rtot = sb4("Rtot", [P, NE], F32)

            key = sb4("key", [P, NE], F32)
            k8 = sb4("k8", [P, 8], F32)
            t8 = sb4("t8", [P, 8], F32)
            t4 = sb4("t4", [P, 4], F32)
            j64 = sb4("j64", [P, NE], F32)
            zrow = sb4("zrow", [P, D], BF16)

            for q4 in range(4):
                S.dma("pool", (lambda q4=q4: lambda e: e.dma_start(
                    out=wo[:, q4 * 4:(q4 + 1) * 4, :],
                    in_=w_out[q4 * 512:(q4 + 1) * 512, :].rearrange("(k p) n -> p k n", p=P)))(),
                    "wo%d" % (q4 % 2), writes=[("wo", q4)])
            S.dma("pool", lambda e: e.dma_start(out=wr[:, :, :], in_=w_router.rearrange("(k p) n -> p k n", p=P)),
                  "c3", writes=["wr"])
            bcast_load(rbb[:, :], router_bias[0:1, :], "b0", "rbb")
            S.op("pool", lambda e: e.memset(Rb[:, :], 0.0), writes=["Rb"])
            S.op("pool", lambda e: e.memset(Rtot[:, :], 0.0), writes=["Rtot"])
            S.op("pool", lambda e: e.memset(zrow[:, :], 0.0), writes=["zrow"])
            S.dma("sp", lambda e: e.dma_start(out=YG_d[DUMP:DUMP + 128, :], in_=zrow[:, :]), "c2",
                  reads=["zrow"], writes=["YGdump"])
            TMPALL = [("tmpc", c) for c in range(4)]
            bcast_load(tmp[:, :], nrm[1:2, :], "b1", TMPALL)
            S.dma("sp", lambda e: e.dma_start(out=gt1g[:, :], in_=modD[0:1, 2 * D:3 * D].partition_broadcast(P)), "b2",
                  writes=["gt1g"])
            S.op("dve", lambda e: e.tensor_tensor(out=gt1g[:, :], in0=gt1g[:, :], in1=tmp[:, :], op=ALU.mult),
                 reads=["gt1g"] + TMPALL, writes=["gt1g"])
            bcast_load(tmp[:, :], nrm[2:3, :], "b1", TMPALL)
            S.dma("sp", lambda e: e.dma_start(out=gm2[:, :], in_=modD[0:1, 4 * D:5 * D].partition_broadcast(P)), "b3",
                  writes=["gm2"])
            S.op("dve", lambda e: e.scalar_tensor_tensor(out=gm2[:, :], in0=gm2[:, :], scalar=1.0, in1=tmp[:, :],
                                                         op0=ALU.add, op1=ALU.mult),
                 reads=["gm2"] + TMPALL, writes=["gm2"])
            S.dma("sp", lambda e: e.dma_start(out=sh2[:, :], in_=modD[0:1, 3 * D:4 * D].partition_broadcast(P)), "b4",
                  writes=["sh2"])

            def A_ld(n):
                s = n % 2
                S.dma("sp", (lambda n=n, s=s: lambda e: e.dma_start(out=yb[s][:, :], in_=Y_d[n * P:(n + 1) * P, :]))(),
                      "yb%d" % s, reads=[("Y_d", n, 0), ("Y_d", n, 1)], writes=[("yb", s)])
                S.dma("sp", (lambda n=n, s=s: lambda e: e.dma_start(out=xt[s][:, :], in_=x[n * P:(n + 1) * P, :]))(),
                      "xt%d" % s, writes=[("xt", s)])

            def A_pe(n):
                s = n % 2
                transposes(lambda i, s=s: yb[s][:, i * 128:(i + 1) * 128], KD,
                           lambda g0, nn: yTt[:, g0:g0 + nn, :], [("yb", s)], lambda g0: ("yTt", g0))
                for cg in range(4):
                    for k in range(KD):
                        S.op("pe", (lambda cg=cg, k=k: lambda e: e.matmul(
                            psM[cg][:, :], lhsT=yTt[:, k, :], rhs=wo[:, k, cg * 512:(cg + 1) * 512],
                            start=(k == 0), stop=(k == KD - 1)))(),
                            reads=[("yTt", 4 * (k // 4)), ("wo", k // 4)], writes=[RM[cg]], inc=(k == KD - 1))
            def A_ew(n):
                s = n % 2
                for cg in range(4):
                    S.op("act", (lambda cg=cg: lambda e: e.activation(
                        out=junk[:, cg * 512:(cg + 1) * 512], in_=psM[cg][:, :], func=AF.Square,
                        accum_out=t4[:, cg:cg + 1]))(),
                        reads=[RM[cg]], writes=[("junk", cg), ("t4", cg)])
                S.op("dve", lambda e: e.tensor_reduce(out=ssq[:, :], in_=t4[:, 0:4], axis=mybir.AxisListType.X, op=ALU.add),
                     reads=[("t4", c) for c in range(4)], writes=["ssq"])
                S.op("act", lambda e: e.activation(out=ssq[:, :], in_=ssq[:, :], func=AF.Sqrt, bias=epsb[:, :], scale=1.0 / D),
                     reads=["ssq", "epsb"], writes=["ssq"])
                S.op("dve", lambda e: e.reciprocal(out=rstd[:, :], in_=ssq[:, :]), reads=["ssq"], writes=["rstd"])
                for cg in range(4):
                    S.op("dve", (lambda cg=cg: lambda e: e.scalar_tensor_tensor(
                        out=tmp[:, cg * 512:(cg + 1) * 512], in0=psM[cg][:, :], scalar=rstd[:, 0:1],
                        in1=gt1g[:, cg * 512:(cg + 1) * 512], op0=ALU.mult, op1=ALU.mult))(),
                        reads=[RM[cg], "rstd", "gt1g"], writes=[("tmpc", cg)])
                S.op("pool", (lambda s=s: lambda e: e.tensor_tensor(out=xt[s][:, :], in0=xt[s][:, :], in1=tmp[:, :], op=ALU.add))(),
                     reads=TMPALL + [("xt", s)], writes=[("xt", s)])
                S.dma("sp", (lambda n=n, s=s: lambda e: e.dma_start(out=X1_d[n * P:(n + 1) * P, :], in_=xt[s][:, :]))(),
                      "x1%d" % s, reads=[("xt", s)], writes=[("X1_d", n)])
            def B_h2(n):
                s = n % 2
                S.op("act", (lambda s=s: lambda e: e.activation(out=junkB[:, :], in_=xt[s][:, :], func=AF.Square, accum_out=ssqB[:, :]))(),
                     reads=[("xt", s)], writes=[("junkB", c) for c in range(4)] + ["ssqB"])
                S.op("act", lambda e: e.activation(out=ssqB[:, :], in_=ssqB[:, :], func=AF.Sqrt, bias=epsb[:, :], scale=1.0 / D),
                     reads=["ssqB", "epsb"], writes=["ssqB"])
                S.op("dve", lambda e: e.reciprocal(out=rstdB[:, :], in_=ssqB[:, :]), reads=["ssqB"], writes=["rstdB"])
                S.op("dve", (lambda s=s: lambda e: e.scalar_tensor_tensor(
                    out=tmpB[:, :], in0=xt[s][:, :], scalar=rstdB[:, 0:1], in1=gm2[:, :], op0=ALU.mult, op1=ALU.mult))(),
                    reads=[("xt", s), "rstdB", "gm2"], writes=TMPB)
                S.op("pool", (lambda s=s: lambda e: e.tensor_tensor(out=h2b[s][:, :], in0=tmpB[:, :], in1=sh2[:, :], op=ALU.add))(),
                     reads=TMPB + ["sh2"], writes=[("h2b", s)])
                S.dma("sp", (lambda n=n, s=s: lambda e: e.dma_start(out=H_d[n * P:(n + 1) * P, :], in_=h2b[s][:, :]))(),
                      "hd%d" % s, reads=[("h2b", s)], writes=[("H_d", n)])
            def B_pe(n):
                s = n % 2
                transposes(lambda i, s=s: h2b[s][:, i * 128:(i + 1) * 128], KD,
                           lambda g0, nn: h2Tt[:, g0:g0 + nn, :], [("h2b", s)], lambda g0: ("h2Tt", g0))
                pr = psM[4]
                for k in range(KD):
                    S.op("pe", (lambda k=k: lambda e: e.matmul(
                        pr[:, 0:NE], lhsT=h2Tt[:, k, :], rhs=wr[:, k, :], start=(k == 0), stop=(k == KD - 1)))(),
                        reads=[("h2Tt", 4 * (k // 4)), "wr"], writes=[RM[4]], inc=(k == KD - 1))
            def B_rt(n):
                s = n % 2
                pr = psM[4]
                S.op("act", lambda e: e.activation(out=sc[:, :], in_=pr[:, 0:NE], func=AF.Sigmoid), reads=[RM[4]], writes=["sc"])
                S.op("dve", lambda e: e.tensor_tensor(out=sel[:, :], in0=sc[:, :], in1=rbb[:, :], op=ALU.add),
                     reads=["sc", "rbb"], writes=["sel"])
                for g in range(8):
                    S.op("dve", (lambda g=g: lambda e: e.max(out=m8g[:, g * 8:(g + 1) * 8], in_=sel[:, g * 8:(g + 1) * 8]))(),
                         reads=["sel"], writes=[("m8g", g)])
                m8g3 = m8g[:, :].rearrange("p (g k) -> p g k", k=8)
                S.op("dve", lambda e: e.tensor_tensor(out=gs_[:, :], in0=m8g3[:, :, 0], in1=m8g3[:, :, 1], op=ALU.add),
                     reads=[("m8g", g) for g in range(8)], writes=["gs"])
                S.op("dve", lambda e: e.max(out=gm8[:, :], in_=gs_[:, :]), reads=["gs"], writes=["gm8"])
                S.op("dve", lambda e: e.tensor_scalar(out=gmask[:, :], in0=gs_[:, :], scalar1=gm8[:, 3:4], scalar2=None, op0=ALU.is_ge),
                     reads=["gs", "gm8"], writes=["gmask"])
                for g in range(8):
                    S.op("dve", (lambda g=g: lambda e: e.tensor_scalar(
                        out=tsel[:, g * 8:(g + 1) * 8], in0=sel[:, g * 8:(g + 1) * 8], scalar1=1.0, scalar2=gmask[:, g:g + 1],
                        op0=ALU.add, op1=ALU.mult))(),
                        reads=["sel", "gmask"], writes=[("tsel", g)])
                S.op("dve", lambda e: e.max(out=m8[:, :], in_=tsel[:, :]), reads=[("tsel", g) for g in range(8)], writes=["m8"])
                selm_n = selmA[:, n * NE:(n + 1) * NE]
                S.op("dve", (lambda selm_n=selm_n: lambda e: e.tensor_scalar(
                    out=selm_n, in0=tsel[:, :], scalar1=m8[:, 7:8], scalar2=None, op0=ALU.is_ge))(),
                     reads=[("tsel", g) for g in range(8)] + ["m8"], writes=[("selmA", n)])
                S.op("dve", (lambda selm_n=selm_n: lambda e: e.scalar_tensor_tensor(
                    out=Gm[:, :], in0=sc[:, :], scalar=1.0, in1=selm_n, op0=ALU.mult, op1=ALU.mult, accum_out=den[:, :]))(),
                     reads=["sc", ("selmA", n)], writes=["Gm", "den"])
                S.op("dve", lambda e: e.reciprocal(out=den[:, :], in_=den[:, :]), reads=["den"], writes=["den"])
                S.op("dve", (lambda n=n: lambda e: e.tensor_scalar(
                    out=GtA[:, n * NE:(n + 1) * NE], in0=Gm[:, :], scalar1=den[:, 0:1], scalar2=2.5, op0=ALU.mult, op1=ALU.mult))(),
                     reads=["Gm", "den"], writes=[("GtA", n)])
                S.op("pool", (lambda selm_n=selm_n: lambda e: e.tensor_tensor(out=Rtot[:, :], in0=Rtot[:, :], in1=selm_n, op=ALU.add))(),
                     reads=["Rtot", ("selmA", n)], writes=["Rtot"])

            A_ld(0)
            A_ld(1)
            A_pe(0)
            A_ew(0)
            for n in range(NT):
                if n + 1 < NT:
                    A_pe(n + 1)
                B_h2(n)
                if n + 2 < NT:
                    A_ld(n + 2)
                if n + 1 < NT:
                    A_ew(n + 1)
                B_pe(n)
                B_rt(n)

            RT_d = scratch("RT_d", [P, 2 * NT * NE + NE], F32)
            nc_rt[0] = RT_d
            S.dma("sp", lambda e: e.dma_start(out=RT_d[:, 0:NT * NE], in_=selmA[:, :]), "c2",
                  reads=[("selmA", n) for n in range(NT)], writes=["RT0"])
            S.dma("sp", lambda e: e.dma_start(out=RT_d[:, NT * NE:2 * NT * NE], in_=GtA[:, :]), "c2",
                  reads=[("GtA", n) for n in range(NT)], writes=["RT1"])
            S.dma("sp", lambda e: e.dma_start(out=RT_d[:, 2 * NT * NE:], in_=Rtot[:, :]), "c2",
                  reads=["Rtot"], writes=["RT2"])
            with nc.Block() as block:
                S.replay(block, final=(stop <= 4))
        if stop <= 4:
            return nc
        S.fence()

        es5 = ExitStack()
        with es5:
            def sb5(name, shape, dt):
                return es5.enter_context(nc.sbuf_tensor("e5_" + name, list(shape), dt))
            wg = [sb5("wg%d" % i, [P, KD, 512], BF16) for i in range(2)]
            wu = [sb5("wu%d" % i, [P, KD, 512], BF16) for i in range(2)]
            wd = [sb5("wd%d" % i, [P, 4, D], BF16) for i in range(2)]
            xg = sb5("xg", [P, NTE, D], BF16)
            xgT = sb5("xgT", [P, KD, CAP], BF16)
            aT = sb5("aTe", [P, 4, CAP], BF16)
            sg = sb5("sg", [P, 512], F32)
            ye = [sb5("ye%d" % i, [P, D], BF16) for i in range(2)]
            selmA = sb5("selmA", [P, NT * NE], F32)
            selmbA = sb5("selmbA", [P, NT * NE], BF16)
            GtA = sb5("GtA", [P, NT * NE], F32)
            Rtot = sb5("Rtot", [P, NE], F32)
            Rb = sb5("Rb", [P, NE], BF16)
            Rtotb = sb5("Rtotb", [P, NE], BF16)
            cpc = sb5("cpc", [P, 1], F32)
            cpr = sb5("cpr", [P, NE], F32)
            GTb = sb5("GTb", [P, NE], BF16)
            rkc = sb5("rkc", [P, 1], F32)
            OHb = sb5("OHb", [P, NE], BF16)
            OHTb = sb5("OHTb", [P, NE], BF16)
            ebd = sb5("ebd", [P, NE], F32)
            caprow = sb5("caprow", [P, NE], F32)
            widf = sb5("widf", [P, NE * 4], F32)
            hsc = [sb5("hsc0", [P, D], BF16)]
            selm2 = sb5("selm2", [P, NE], F32)
            key = sb5("key", [P, NE], F32)
            k8 = sb5("k8", [P, 8], F32)
            t8 = sb5("t8", [P, 8], F32)
            j64 = sb5("j64", [P, NE], F32)
            RT_d = nc_rt[0]
            S.dma("sp", lambda e: e.dma_start(out=selmA[:, :], in_=RT_d[:, 0:NT * NE]), "c2", writes=[("selmA", n) for n in range(NT)])
            S.dma("sp", lambda e: e.dma_start(out=GtA[:, :], in_=RT_d[:, NT * NE:2 * NT * NE]), "c2", writes=[("GtA", n) for n in range(NT)])
            S.dma("sp", lambda e: e.dma_start(out=Rtot[:, :], in_=RT_d[:, 2 * NT * NE:]), "c2", writes=["Rtot"])
            S.op("pool", lambda e: e.memset(Rb[:, :], 0.0), writes=["Rb"])
            S.op("act", lambda e: e.copy(out=selmbA[:, :], in_=selmA[:, :]),
                 reads=[("selmA", n) for n in range(NT)], writes=[("selmbA", n) for n in range(NT)])
            ecol_b = cb[:, NCB0:NCB0 + 128]
            tsb_b = cb[:, NCB0 + 128:NCB0 + 256]
            capb_b = cb[:, NCB0 + 256:NCB0 + 384]
            S.op("dve", lambda e: e.tensor_copy(out=Rtotb[:, :], in_=Rtot[:, :]), reads=["Rtot"], writes=["Rtotb"])
            S.op("pe", lambda e: e.matmul(psM[4][0:NE, 0:1], lhsT=Rtotb[:, :], rhs=ones_b[:, 0:1], start=True, stop=True),
                 reads=["Rtotb", "cb"], writes=[RM[4]])
            S.op("pe", lambda e: e.matmul(psM[5][:, 0:NE], lhsT=ones_b, rhs=Rtotb[:, :], start=True, stop=True),
                 reads=["Rtotb", "cb"], writes=[RM[5]])
            S.op("dve", lambda e: e.tensor_scalar(out=cpc[0:NE, :], in0=psM[4][0:NE, 0:1], scalar1=64.0,
                                                  scalar2=CFs("tbcol")[0:NE, :], op0=ALU.mult, op1=ALU.add),
                 reads=[RM[4], "cf"], writes=["cpc"])
            S.op("dve", lambda e: e.scalar_tensor_tensor(out=cpr[0:NE, :], in0=psM[5][0:NE, 0:NE], scalar=64.0,
                                                         in1=CFs("tbrow")[0:NE, :], op0=ALU.mult, op1=ALU.add),
                 reads=[RM[5], "cf"], writes=["cpr"])
            S.op("dve", lambda e: e.tensor_scalar(out=GTb[0:NE, :], in0=cpr[0:NE, :], scalar1=cpc[0:NE, 0:1], scalar2=None, op0=ALU.is_lt),
                 reads=["cpr", "cpc"], writes=["GTb"])
            S.op("dve", lambda e: e.scalar_tensor_tensor(out=j64[0:NE, :], in0=cpr[0:NE, :], scalar=cpc[0:NE, 0:1],
                                                         in1=CFs("ones64")[0:NE, :], op0=ALU.is_gt, op1=ALU.mult,
                                                         accum_out=rkc[0:NE, :]),
                 reads=["cpr", "cpc", "cf"], writes=["j64", "rkc"])
            S.op("pe", lambda e: e.matmul(psM[4][:, 0:NE], lhsT=ones_b[0:NE, :], rhs=GTb[0:NE, :], start=True, stop=True),
                 reads=["GTb", "cb", "cpc"], writes=[RM[4]])
            S.op("dve", lambda e: e.tensor_scalar(out=OHb[0:NE, :], in0=CFs("iotar")[0:NE, :], scalar1=rkc[0:NE, 0:1], scalar2=None,
                                                  op0=ALU.is_equal),
                 reads=["rkc", "cf"], writes=["OHb"])
            S.op("dve", lambda e: e.tensor_scalar(out=OHTb[0:NE, :], in0=psM[4][0:NE, 0:NE], scalar1=CFs("iotac")[0:NE, :], scalar2=None,
                                                  op0=ALU.is_equal),
                 reads=[RM[4], "cf"], writes=["OHTb"])
            S.op("pe", lambda e: e.matmul(psM[5][:, 0:NE], lhsT=ecol_b[0:NE, :], rhs=OHb[0:NE, :], start=True, stop=True),
                 reads=["OHb", "cb", "cpr"], writes=[RM[5]])
            S.op("pe", lambda e: e.matmul(psM[4][:, 0:NE], lhsT=tsb_b[0:NE, :], rhs=OHTb[0:NE, :], start=True, stop=True),
                 reads=["OHTb", "cb"], writes=[RM[4]])
            S.op("pe", lambda e: e.matmul(psM[3][:, 0:NE], lhsT=capb_b[0:NE, :], rhs=OHTb[0:NE, :], start=True, stop=True),
                 reads=["OHTb", "cb"], writes=[RM[3]])
            S.op("dve", lambda e: e.tensor_scalar(out=ebd[:, :], in0=psM[4][:, 0:NE], scalar1=128.0, scalar2=1.0, op0=ALU.mult, op1=ALU.add),
                 reads=[RM[4]], writes=["ebd"])
            S.op("dve", lambda e: e.tensor_scalar(out=caprow[:, :], in0=psM[3][:, 0:NE], scalar1=128.0, scalar2=None, op0=ALU.mult),
                 reads=[RM[3]], writes=["caprow"])
            widf3 = widf[:, :].rearrange("p (r k) -> p r k", k=4)
            for kq in range(4):
                S.op("dve", (lambda kq=kq: lambda e: e.tensor_scalar(
                    out=widf3[:, :, kq], in0=psM[5][:, 0:NE], scalar1=512.0, scalar2=CFs("kqp")[:, kq:kq + 1],
                    op0=ALU.mult, op1=ALU.add))(),
                    reads=[RM[5], "cf"], writes=[("widf", kq)])
            S.op("dve", lambda e: e.tensor_copy(out=widx[:, :], in_=widf[:, :]), reads=[("widf", kq) for kq in range(4)], writes=["widx"])

            def pass2_tile(n):
                s = 0
                selm_n = selmA[:, n * NE:(n + 1) * NE]
                selmb_n = selmbA[:, n * NE:(n + 1) * NE]
                Gt_n = GtA[:, n * NE:(n + 1) * NE]
                S.dma("sp", (lambda n=n, s=s: lambda e: e.dma_start(out=hsc[s][:, :], in_=H_d[n * P:(n + 1) * P, :]))(),
                      "hs0", reads=[("H_d", n)], writes=[("hsc", s)])
                pp = psM[5]
                S.op("pe", lambda e: e.matmul(pp[:, 0:NE], lhsT=ones_b, rhs=Rb[:, :], start=True, stop=False),
                     reads=["cb", "Rb"], writes=[RM[5]], inc=False)
                S.op("pe", (lambda selmb_n=selmb_n: lambda e: e.matmul(pp[:, 0:NE], lhsT=tri_b, rhs=selmb_n, start=False, stop=True))(),
                     reads=["cb", ("selmbA", n)], writes=[RM[5]])
                S.op("pool", (lambda selmb_n=selmb_n: lambda e: e.tensor_tensor(out=Rb[:, :], in0=Rb[:, :], in1=selmb_n, op=ALU.add))(),
                     reads=["Rb", ("selmbA", n)], writes=["Rb"])
                S.op("dve", lambda e: e.tensor_tensor(out=selm2[:, :], in0=pp[:, 0:NE], in1=caprow[:, :], op=ALU.is_lt),
                     reads=[RM[5], "caprow"], writes=["selm2"])
                S.op("dve", (lambda selm_n=selm_n: lambda e: e.tensor_tensor(out=selm2[:, :], in0=selm2[:, :], in1=selm_n, op=ALU.mult))(),
                     reads=["selm2", ("selmA", n)], writes=["selm2"])
                S.op("dve", lambda e: e.tensor_tensor(out=key[:, :], in0=pp[:, 0:NE], in1=ebd[:, :], op=ALU.add),
                     reads=[RM[5], "ebd"], writes=["key"])
                S.op("dve", lambda e: e.tensor_tensor(out=key[:, :], in0=key[:, :], in1=selm2[:, :], op=ALU.mult),
                     reads=["key", "selm2"], writes=["key"])
                S.op("dve", lambda e: e.max(out=k8[:, :], in_=key[:, :]), reads=["key"], writes=["k8"])
                for k in range(8):
                    S.op("dve", (lambda k=k, n=n, Gt_n=Gt_n: lambda e: e.scalar_tensor_tensor(
                        out=j64[:, :], in0=key[:, :], scalar=k8[:, k:k + 1], in1=Gt_n, op0=ALU.is_equal, op1=ALU.mult,
                        accum_out=gate8[:, n * 8 + k:n * 8 + k + 1]))(),
                        reads=["key", "k8", ("GtA", n)], writes=["j64", ("gate8", n)])
                S.op("dve", lambda e: e.tensor_scalar(out=t8[:, :], in0=k8[:, :], scalar1=0.0, scalar2=float(DUMP + 1),
                                                      op0=ALU.is_equal, op1=ALU.mult),
                     reads=["k8"], writes=["t8b"])
                S.op("dve", lambda e: e.tensor_tensor(out=t8[:, :], in0=t8[:, :], in1=k8[:, :], op=ALU.add),
                     reads=["t8b", "k8"], writes=["t8b"])
                S.op("dve", (lambda n=n: lambda e: e.tensor_scalar(
                    out=dsti[:, n * 8:(n + 1) * 8], in0=t8[:, :], scalar1=-1.0, scalar2=None, op0=ALU.add))(),
                    reads=["t8b"], writes=[("dsti", n)])
                for k in range(8):
                    S.dma("pool", (lambda k=k, n=n, s=s: lambda e: e.indirect_dma_start(
                        out=XG_d, out_offset=bass.IndirectOffsetOnAxis(ap=dsti[:, n * 8 + k:n * 8 + k + 1], axis=0),
                        in_=hsc[s][:, :], in_offset=None))(),
                        "sc%d" % (k % SCK), reads=[("dsti", n), ("hsc", s)], writes=["XG_all"])
            rorder = []
            for i in range(NE // 2):
                rorder += [i, NE - 1 - i]
            items = [("s", i) for i in range(4)] + [("e", r) for r in rorder]
            witems = [it for it, (kind, idx) in enumerate(items) if kind == "e" or idx == 0]

            def load_w(wi):
                kind, idx = items[witems[wi]]
                ws = wi % 2
                if kind == "s":
                    S.dma("pool", lambda e: e.dma_start(out=wg[ws][:, :, :], in_=w_sg.rearrange("(k p) f -> p k f", p=P)),
                          "wg0", writes=[("wg", ws, kq) for kq in range(4)])
                    S.dma("pool", lambda e: e.dma_start(out=wu[ws][:, :, :], in_=w_su.rearrange("(k p) f -> p k f", p=P)),
                          "wu0", writes=[("wu", ws, kq) for kq in range(4)])
                    S.dma("pool", lambda e: e.dma_start(out=wd[ws][:, :, :], in_=w_sd.rearrange("(k p) d -> p k d", p=P)),
                          "wd0", writes=[("wd", ws, kq) for kq in range(4)])
                else:
                    def gath(dst, src, kq):
                        return lambda e: e.indirect_dma_start(
                            out=dst, out_offset=None, in_=src,
                            in_offset=bass.IndirectOffsetOnAxis(ap=widx[:, idx * 4 + kq:idx * 4 + kq + 1], axis=0))
                    for kq in range(4):
                        S.dma("pool", gath(wg[ws][:, kq * 4:(kq + 1) * 4, :].rearrange("p a b -> p (a b)"), w_eg, kq),
                              "wg0", reads=["widx"], writes=[("wg", ws, kq)])
                        S.dma("pool", gath(wu[ws][:, kq * 4:(kq + 1) * 4, :].rearrange("p a b -> p (a b)"), w_eu, kq),
                              "wu0", reads=["widx"], writes=[("wu", ws, kq)])
                        S.dma("pool", gath(wd[ws][:, kq, :], w_ed, kq),
                              "wd0", reads=["widx"], writes=[("wd", ws, kq)])

            def load_x(it):
                kind, idx = items[it]
                if kind == "s":
                    src, nt = H_d[idx * 512:(idx + 1) * 512, :], 4
                else:
                    nt = CAPT[idx]
                    src = XG_d[TSTART[idx] * P:(TSTART[idx] + nt) * P, :]
                S.dma("sp", lambda e: e.dma_start(out=xg[:, 0:nt, :], in_=src.rearrange("(j p) d -> p j d", p=P)),
                      "xg0", reads=(["XG_all"] if kind == "e" else []), writes=["xg"])

            load_w(0)
            load_x(0)
            load_w(1)
            wi = -1
            yec = 0
            for it, (kind, idx) in enumerate(items):
                if kind == "e" or idx == 0:
                    wi += 1
                    if wi >= 1 and wi + 1 < len(witems):
                        load_w(wi + 1)
                ws = wi % 2
                if kind == "s":
                    for n2 in range(4 * idx, 4 * idx + 4):
                        pass2_tile(n2)
                nt = 4 if kind == "s" else CAPT[idx]
                nrow = nt * P
                groups = [(0, nrow)] if nrow <= 512 else [(0, nrow // 2), (nrow // 2, nrow)]
                tcount = 0
                for k in range(KD):
                    for j0 in range(0, nt, 4):
                        nj = min(4, nt - j0)
                        bank = tcount % 2
                        tcount += 1
                        base = 0
                        for jj in range(nj):
                            j = j0 + jj
                            S.op("pe", (lambda k=k, j=j, jj=jj, bank=bank, base=base: lambda e: e.transpose(
                                out=psT[bank][:, base + jj * 128:base + (jj + 1) * 128], in_=xg[:, j, k * 128:(k + 1) * 128],
                                identity=ident))(),
                                reads=["xg", "cb"], writes=[("psT", bank)], inc=(jj == nj - 1))
                        copy_op(evac_eng(), xgT[:, k, j0 * 128:(j0 + nj) * 128], psT[bank][:, base:base + nj * 128],
                                reads=[("psT", bank)], writes=[("xgT", k, j0)])
                if it + 1 < len(items):
                    load_x(it + 1)
                xr = lambda k: [("xgT", k, j0) for j0 in range(0, nt, 4)]
                for (r0, r1) in groups:
                    nr = r1 - r0
                    for fc in range(4):
                        for (wt_, pm_i, wn) in ((wg, 4, "wg"), (wu, 5, "wu")):
                            for k in range(KD):
                                S.op("pe", (lambda k=k, fc=fc, wt_=wt_, pm_i=pm_i, ws=ws, r0=r0, r1=r1, nr=nr: lambda e: e.matmul(
                                    psM[pm_i][:, 0:nr], lhsT=wt_[ws][:, k, fc * 128:(fc + 1) * 128], rhs=xgT[:, k, r0:r1],
                                    start=(k == 0), stop=(k == KD - 1)))(),
                                    reads=[(wn, ws, k // 4)] + xr(k), writes=[RM[pm_i]], inc=(k == KD - 1))
                        S.op("act", (lambda nr=nr: lambda e: e.activation(out=sg[:, 0:nr], in_=psM[4][:, 0:nr], func=AF.Silu))(),
                             reads=[RM[4]], writes=["sg"])
                        S.op("dve", (lambda fc=fc, r0=r0, r1=r1, nr=nr: lambda e: e.tensor_tensor(
                            out=aT[:, fc, r0:r1], in0=sg[:, 0:nr], in1=psM[5][:, 0:nr], op=ALU.mult))(),
                            reads=["sg", RM[5]], writes=[("aT", fc, r0)])
                ar = lambda fc: [("aT", fc, r0) for (r0, r1) in groups]
                for j in range(nt):
                    for dc in range(4):
                        for fc in range(4):
                            S.op("pe", (lambda j=j, dc=dc, fc=fc, ws=ws: lambda e: e.matmul(
                                psM[dc][:, :], lhsT=aT[:, fc, j * 128:(j + 1) * 128], rhs=wd[ws][:, fc, dc * 512:(dc + 1) * 512],
                                start=(fc == 0), stop=(fc == 3)))(),
                                reads=ar(fc) + [("wd", ws, fc)], writes=[RM[dc]], inc=(fc == 3))
                    ys = yec % 2
                    yec += 1
                    for dc in range(4):
                        copy_op("act" if dc % 2 == 0 else "dve", ye[ys][:, dc * 512:(dc + 1) * 512], psM[dc][:, :],
                                reads=[RM[dc]], writes=[("ye", ys, dc)])
                    if kind == "s":
                        dst = YS_d[idx * 512 + j * P:idx * 512 + (j + 1) * P, :]
                        wn_ = ("YS_d", idx * 4 + j)
                    else:
                        dst = YG_d[(TSTART[idx] + j) * P:(TSTART[idx] + j + 1) * P, :]
                        wn_ = ("YG_d", idx, j)
                    S.dma("sp", (lambda ys=ys, dst=dst: lambda e: e.dma_start(out=dst, in_=ye[ys][:, :]))(),
                          "ye%d" % ys, reads=[("ye", ys, dc) for dc in range(4)], writes=[wn_])
            with nc.Block() as block:
                S.replay(block, final=(stop <= 5))
        if stop <= 5:
            return nc
        S.fence()

        es6 = ExitStack()
        with es6:
            def sb6(name, shape, dt):
                return es6.enter_context(nc.sbuf_tensor("e6_" + name, list(shape), dt))
            xt = [sb6("xt%d" % i, [P, D], F32) for i in range(2)]
            gk = [sb6("gk%d" % i, [P, D], BF16) for i in range(16)]
            ysb = [sb6("ysb%d" % i, [P, D], BF16) for i in range(2)]
            dg = [sb6("dg%d" % i, [P, 8 * P], BF16) for i in range(2)]
            gt2g = sb6("gt2g", [P, D], F32)
            tmp = sb6("tmp", [P, D], F32)
            junk = sb6("junk", [P, D], BF16)
            t4 = sb6("t4", [P, 4], F32)
            ssq = sb6("ssq", [P, 1], F32)
            rstd = sb6("rstd", [P, 1], F32)
            TMP5 = [("tmp5", c) for c in range(4)]
            bcast_load(tmp[:, :], nrm[3:4, :], "b1", TMP5)
            S.dma("sp", lambda e: e.dma_start(out=gt2g[:, :], in_=modD[0:1, 5 * D:6 * D].partition_broadcast(P)), "b2",
                  writes=["gt2g"])
            S.op("dve", lambda e: e.tensor_tensor(out=gt2g[:, :], in0=gt2g[:, :], in1=tmp[:, :], op=ALU.mult),
                 reads=["gt2g"] + TMP5, writes=["gt2g"])
            for k in range(16):
                S.op("pool", (lambda k=k: lambda e: e.memset(gk[k][:, :], 0.0))(), writes=[("gk", k)])
            def c_load(n):
                s = n % 2
                S.dma("sp", lambda e: e.dma_start(out=ysb[s][:, :], in_=YS_d[n * P:(n + 1) * P, :]),
                      "ac%d" % s, writes=[("ysb", s)])
                S.dma("sp", lambda e: e.dma_start(out=xt[s][:, :], in_=X1_d[n * P:(n + 1) * P, :]),
                      "xt%d" % s, writes=[("xt", s)])

            c_load(0)
            for n in range(NT):
                s = n % 2
                if n + 1 < NT:
                    c_load(n + 1)
                for k in range(8):
                    S.dma("pool", (lambda k=k, n=n, s=s: lambda e: e.indirect_dma_start(
                        out=gk[s * 8 + k][:, :], out_offset=None, in_=YG_d,
                        in_offset=bass.IndirectOffsetOnAxis(ap=dsti[:, n * 8 + k:n * 8 + k + 1], axis=0)))(),
                        "gk%d" % (k % 3), writes=[("gk", s * 8 + k)])
                    S.op("dve", (lambda k=k, n=n, s=s: lambda e: e.tensor_scalar(
                        out=dg[s][:, k * P:(k + 1) * P], in0=ident, scalar1=gate8[:, n * 8 + k:n * 8 + k + 1], scalar2=None,
                        op0=ALU.mult))(),
                        reads=["cb"], writes=[("dg", s, k)])
                for c in range(4):
                    S.op("pe", (lambda c=c, s=s: lambda e: e.matmul(
                        psM[c][:, :], lhsT=ident, rhs=ysb[s][:, c * 512:(c + 1) * 512], start=True, stop=False))(),
                        reads=["cb", ("ysb", s)], writes=[RM[c]], inc=False)
                    for k in range(8):
                        S.op("pe", (lambda c=c, k=k, s=s: lambda e: e.matmul(
                            psM[c][:, :], lhsT=dg[s][:, k * P:(k + 1) * P], rhs=gk[s * 8 + k][:, c * 512:(c + 1) * 512],
                            start=False, stop=(k == 7)))(),
                            reads=[("dg", s, k), ("gk", s * 8 + k)], writes=[RM[c]], inc=(k == 7))
                for c in range(4):
                    S.op("act", (lambda c=c: lambda e: e.activation(
                        out=junk[:, c * 512:(c + 1) * 512], in_=psM[c][:, :], func=AF.Square, accum_out=t4[:, c:c + 1]))(),
                        reads=[RM[c]], writes=[("junk5", c), ("t45", c)])
                S.op("dve", lambda e: e.tensor_reduce(out=ssq[:, :], in_=t4[:, 0:4], axis=mybir.AxisListType.X, op=ALU.add),
                     reads=[("t45", c) for c in range(4)], writes=["ssq"])
                S.op("act", lambda e: e.activation(out=ssq[:, :], in_=ssq[:, :], func=AF.Sqrt, bias=epsb[:, :], scale=1.0 / D),
                     reads=["ssq", "epsb"], writes=["ssq"])
                S.op("dve", lambda e: e.reciprocal(out=rstd[:, :], in_=ssq[:, :]), reads=["ssq"], writes=["rstd"])
                for c in range(4):
                    S.op("dve", (lambda c=c: lambda e: e.scalar_tensor_tensor(
                        out=tmp[:, c * 512:(c + 1) * 512], in0=psM[c][:, :], scalar=rstd[:, 0:1],
                        in1=gt2g[:, c * 512:(c + 1) * 512], op0=ALU.mult, op1=ALU.mult))(),
                        reads=[RM[c], "rstd", "gt2g"], writes=[("tmp5", c)])
                S.op("dve", (lambda s=s: lambda e: e.tensor_tensor(out=xt[s][:, :], in0=xt[s][:, :], in1=tmp[:, :], op=ALU.add))(),
                     reads=TMP5 + [("xt", s)], writes=[("xt", s)])
                S.dma("sp", (lambda n=n, s=s: lambda e: e.dma_start(out=out[n * P:(n + 1) * P, :], in_=xt[s][:, :]))(),
                      "ou%d" % s, reads=[("xt", s)], writes=[("out", n)])
            with nc.Block() as block:
                S.replay(block, final=True)
    return nc


_CONSTS = None


def _relay(w):
    return np.ascontiguousarray(w.reshape(NE, 4, 4, 128, 512).transpose(0, 1, 3, 2, 4)).reshape(NE * 512, D)


def _prep_inputs(inputs):
    global _CONSTS
    if _CONSTS is None:
        _CONSTS = _make_consts()
    cf, cbm = _CONSTS
    f = lambda a: np.ascontiguousarray(np.asarray(a, dtype=np.float32))
    x = f(inputs["x"]); ctx = f(inputs["ctx"]); c = f(inputs["c"]); c_ctx = f(inputs["c_ctx"])
    wa2 = np.zeros((33, 1024), np.float32)
    wa2[0:16, 0:512] = f(inputs["w_a2_fwd"])[0]
    wa2[16:32, 512:1024] = f(inputs["w_a2_bwd"])[0]
    wa2[32, 0:512] = f(inputs["b_a_fwd"])[0]
    wa2[32, 512:1024] = f(inputs["b_a_bwd"])[0]
    nrm = np.concatenate([f(inputs[k]) for k in ("norm_mix_pre", "norm_mix_post", "norm_ffn_pre", "norm_ffn_post")], axis=0)
    shared = dict(
        w_mod=f(inputs["w_mod"])[0], b_mod=f(inputs["b_mod"]), nrm=nrm, w_in=f(inputs["w_in"])[0], wa2=wa2,
        gla_norm=f(inputs["gla_norm"]), w_pool=f(inputs["w_pool"])[0], pool_scale=f(inputs["pool_scale"]),
        w_out=f(inputs["w_out"])[0], w_router=f(inputs["w_router"])[0], router_bias=f(inputs["router_bias"]),
        w_eg=_relay(f(inputs["w_exp_gate"])[0]), w_eu=_relay(f(inputs["w_exp_up"])[0]),
        w_ed=f(inputs["w_exp_down"])[0].reshape(NE * 512, D),
        w_sg=f(inputs["w_sh_gate"])[0], w_su=f(inputs["w_sh_up"])[0], w_sd=f(inputs["w_sh_down"])[0],
        cf=cf, cb=cbm)
    maps = []
    for b in range(8):
        ccv = np.stack([c[b], c_ctx], axis=-1).reshape(16, 128, 2).transpose(1, 0, 2).reshape(128, 32)
        m = dict(shared)
        m.update(x=x[b], ctx=ctx[b], cc=np.ascontiguousarray(ccv))
        maps.append(m)
    return maps


def kernel(**inputs):
    maps = _prep_inputs(inputs)
    nc = build()
    res = run_bass_kernel_spmd(nc, maps, core_ids=list(range(8)))
    return np.stack([np.asarray(r["out"], dtype=np.float32) for r in res.results], axis=0)
```
